# Optimizing a Trainium2 kernel written in Bass

```python
import math
import jax
import jax.numpy as jnp
from jax import lax
import numpy as np

D_MODEL = 1024
BATCH = 16
SEQ = 2048
DEPTH = 2

GRID_W = 64
CTX_LEN = 256
D_MIX = D_MODEL
BRANCH = D_MIX // 4
HEAD_DIM = 64
BRANCH_HEADS = BRANCH // HEAD_DIM
SSD_HEADS = BRANCH_HEADS
SSD_GROUPS = 2
SSD_STATE = 128
SSD_BC = SSD_GROUPS * SSD_STATE
SSD_CONV = 5
SSD_CONV_CH = BRANCH + 2 * SSD_BC
SSD_CHUNK = 2 * GRID_W
GM_CHUNK = 2 * GRID_W
GM_HEADS = BRANCH_HEADS
GM_HEAD_DIM = BRANCH // GM_HEADS
SC_CONV = 3
CF_CONV = 31
D_FF = 2816
N_EXPERTS = 8
TOP_K = 2
E_FF = 3584
N_DENSE = (DEPTH + 1) // 2
N_MOE = DEPTH // 2
IN_WIDTHS = (SSD_CONV_CH, 2 * SSD_HEADS, BRANCH, 2 * BRANCH, 3 * BRANCH, 2 * BRANCH)
IN_COLS = sum(IN_WIDTHS)
IN_SPLITS = tuple(int(s) for s in np.cumsum(IN_WIDTHS)[:-1])
SSD_CTX_COLS = SSD_CONV_CH + 2 * SSD_HEADS

kernel_name = 'hybrid_ssd_gmlp_conv_moe_dit'


def rmsnorm(x, g, eps=1e-6):
    xf = x.astype(jnp.float32)
    y = xf * lax.rsqrt(jnp.mean(xf * xf, axis=-1, keepdims=True) + eps)
    return y.astype(x.dtype) * g


def layernorm(x, g, b, eps=1e-5):
    xf = x.astype(jnp.float32)
    mu = jnp.mean(xf, axis=-1, keepdims=True)
    var = jnp.mean(jnp.square(xf - mu), axis=-1, keepdims=True)
    return ((xf - mu) * lax.rsqrt(var + eps)).astype(x.dtype) * g + b


def modulate(h, shift, scale):
    return h * (1 + scale) + shift


def dwconv(x, w):
    k, ch = w.shape
    return lax.conv_general_dilated(x, w[:, None, :], (1,), [(k // 2, k // 2)],
                                    dimension_numbers=('NWC', 'WIO', 'NWC'), feature_group_count=ch)


def flip(t):
    return jnp.flip(t, axis=1)


def to_chunks(t):
    bsz, length = t.shape[:2]
    return t.reshape(bsz, length // SSD_CHUNK, SSD_CHUNK, *t.shape[2:])


def ssd_prepare(xs, dt, a):
    bsz, length, nh, hd = xs.shape
    nc = length // SSD_CHUNK
    xdt = (xs * dt[..., None]).reshape(bsz, nc, SSD_CHUNK, nh, hd)
    a_cs = jnp.cumsum((dt * a).reshape(bsz, nc, SSD_CHUNK, nh), axis=2).transpose(0, 3, 1, 2)
    return xdt, a_cs


def ssd_chunk_states(xdt, a_cs, b_c, init):
    decay_to_end = jnp.exp(a_cs[..., -1:] - a_cs)
    chunk_states = jnp.einsum('bclhn,bhcl,bclhp->bchpn', b_c, decay_to_end, xdt)
    chunk_decay = jnp.exp(a_cs[..., -1])

    def step(state, inp):
        s_c, d_c = inp
        return state * d_c[..., None, None] + s_c, state

    final, entering = lax.scan(step, init, (jnp.moveaxis(chunk_states, 1, 0), jnp.moveaxis(chunk_decay, 2, 0)))
    return jnp.moveaxis(entering, 0, 1), final


def ssd_scan(xs, dt, a, bs, cs, init):
    xdt, a_cs = ssd_prepare(xs, dt, a)
    b_c, c_c = to_chunks(bs), to_chunks(cs)
    entering, final = ssd_chunk_states(xdt, a_cs, b_c, init)
    lower = jnp.tril(jnp.ones((SSD_CHUNK, SSD_CHUNK), dtype=bool))
    lmat = jnp.exp(jnp.where(lower, a_cs[..., :, None] - a_cs[..., None, :], -jnp.inf))
    y_diag = jnp.einsum('bclhn,bcshn,bhcls,bcshp->bclhp', c_c, b_c, lmat, xdt)
    y_off = jnp.einsum('bclhn,bchpn,bhcl->bclhp', c_c, entering, jnp.exp(a_cs))
    return (y_diag + y_off).reshape(xs.shape), final


def ssd_inputs(xbc_raw, dt_raw, p):
    bsz, length, _ = xbc_raw.shape
    xbc = jax.nn.silu(dwconv(xbc_raw, p['ssd_conv_w']) + p['ssd_conv_b']).astype(jnp.float32)
    xs, bs, cs = jnp.split(xbc, (BRANCH, BRANCH + SSD_BC), axis=-1)
    rep = SSD_HEADS // SSD_GROUPS
    xs = xs.reshape(bsz, length, SSD_HEADS, HEAD_DIM)
    bs = jnp.repeat(bs.reshape(bsz, length, SSD_GROUPS, SSD_STATE), rep, axis=2)
    cs = jnp.repeat(cs.reshape(bsz, length, SSD_GROUPS, SSD_STATE), rep, axis=2)
    dt = jax.nn.softplus(dt_raw.astype(jnp.float32).reshape(bsz, length, 2, SSD_HEADS)
                         + p['ssd_dt_bias'].astype(jnp.float32))
    a = -jnp.exp(p['ssd_a_log'].astype(jnp.float32))
    return xs, bs, cs, dt, a


def ssd_bidir(xs, bs, cs, dt, a, init_f, init_b):
    y_f, fin_f = ssd_scan(xs, dt[:, :, 0], a[0], bs, cs, init_f)
    y_b, fin_b = ssd_scan(flip(xs), flip(dt[:, :, 1]), a[1], flip(bs), flip(cs), init_b)
    return y_f + flip(y_b), fin_f, fin_b


def ssd_context_states(zc, p, init):
    xbc_raw, dt_raw = jnp.split(zc, (SSD_CONV_CH,), axis=-1)
    xs, bs, _, dt, a = ssd_inputs(xbc_raw, dt_raw, p)
    xdt_f, acs_f = ssd_prepare(xs, dt[:, :, 0], a[0])
    fin_f = ssd_chunk_states(xdt_f, acs_f, to_chunks(bs), init)[1]
    xdt_b, acs_b = ssd_prepare(flip(xs), flip(dt[:, :, 1]), a[1])
    fin_b = ssd_chunk_states(xdt_b, acs_b, to_chunks(flip(bs)), init)[1]
    return fin_f, fin_b


def mixer_branches(z, p, init_f, init_b):
    xbc_raw, dt_raw, ssd_z, uv, sc, cf = jnp.split(z, IN_SPLITS, axis=-1)
    bsz, length, _ = z.shape
    xs, bs, cs, dt, a = ssd_inputs(xbc_raw, dt_raw, p)
    y, fin_f, fin_b = ssd_bidir(xs, bs, cs, dt, a, init_f, init_b)
    y = (y + p['ssd_d'].astype(jnp.float32)[:, None] * xs).reshape(bsz, length, BRANCH).astype(z.dtype)
    y_ssd = rmsnorm(y * jax.nn.silu(ssd_z), p['ssd_norm_g'])
    u, v = jnp.split(jax.nn.gelu(uv), 2, axis=-1)
    v = layernorm(v, p['gm_norm_g'], p['gm_norm_b'])
    v = v.reshape(bsz, length // GM_CHUNK, GM_CHUNK, GM_HEADS, GM_HEAD_DIM)
    s = jnp.einsum('gts,bcsgd->bctgd', p['gm_ws'], v) + p['gm_bs'].T[:, :, None]
    y_gm = u * s.reshape(bsz, length, BRANCH)
    gb, gc, xin = jnp.split(sc, 3, axis=-1)
    y_sc = gb * dwconv(gc * xin, p['sc_conv_w'])
    ca, cg = jnp.split(cf, 2, axis=-1)
    cv = dwconv(ca * jax.nn.sigmoid(cg), p['cf_conv_w']) + p['cf_conv_b']
    y_cf = jax.nn.silu(layernorm(cv, p['cf_norm_g'], p['cf_norm_b']))
    return jnp.concatenate([y_ssd, y_gm, y_sc, y_cf], axis=-1), fin_f, fin_b


def swiglu(h, w1, w3, w2):
    return (jax.nn.silu(h @ w1) * (h @ w3)) @ w2


def moe_swiglu(h, router, w1, w3, w2):
    logits = (h @ router).astype(jnp.float32)
    top_v, top_i = lax.top_k(logits, TOP_K)
    probs = jax.nn.softmax(top_v, axis=-1)
    combine = jnp.sum(jax.nn.one_hot(top_i, N_EXPERTS, dtype=jnp.float32) * probs[..., None], axis=-2).astype(h.dtype)
    out = jnp.zeros_like(h)
    for e in range(N_EXPERTS):
        out = out + combine[..., e:e + 1] * swiglu(h, w1[e], w3[e], w2[e])
    return out


def channel_mixer(t, l, ffn_w1, ffn_w3, ffn_w2, moe_router, moe_w1, moe_w3, moe_w2):
    i = l // 2
    if l % 2 == 0:
        return swiglu(t, ffn_w1[i], ffn_w3[i], ffn_w2[i])
    return moe_swiglu(t, moe_router[i], moe_w1[i], moe_w3[i], moe_w2[i])


def setup_inputs(seed: int = 0) -> dict:
    key = jax.random.key(seed)
    ks = iter(jax.random.split(key, 48))

    def nrm(shape, scale):
        return jax.random.normal(next(ks), shape, jnp.float32) * scale

    def gain(shape):
        return 1.0 + nrm(shape, 0.05)

    dt0 = jnp.exp(jax.random.uniform(next(ks), (DEPTH, 2, SSD_HEADS), jnp.float32, math.log(1e-3), math.log(1e-1)))
    a0 = jax.random.uniform(next(ks), (DEPTH, 2, SSD_HEADS), jnp.float32, 1.0, 16.0)
    return {
        'x': nrm((BATCH, SEQ, D_MODEL), 1.0),
        'c': nrm((BATCH, D_MODEL), 1.0),
        'ctx': nrm((BATCH, CTX_LEN, D_MODEL), 1.0),
        'c_ctx': nrm((D_MODEL,), 1.0),
        'mod_w': nrm((DEPTH, D_MODEL, 6 * D_MODEL), 0.5 * D_MODEL ** -0.5),
        'mod_b': nrm((DEPTH, 6 * D_MODEL), 0.02),
        'norm1_g': gain((DEPTH, D_MODEL)),
        'norm2_g': gain((DEPTH, D_MODEL)),
        'w_in': nrm((DEPTH, D_MODEL, IN_COLS), D_MODEL ** -0.5),
        'w_out': nrm((DEPTH, D_MIX, D_MODEL), D_MIX ** -0.5),
        'ssd_conv_w': nrm((DEPTH, SSD_CONV, SSD_CONV_CH), SSD_CONV ** -0.5),
        'ssd_conv_b': nrm((DEPTH, SSD_CONV_CH), 0.02),
        'ssd_dt_bias': dt0 + jnp.log(-jnp.expm1(-dt0)),
        'ssd_a_log': jnp.log(a0),
        'ssd_d': gain((DEPTH, SSD_HEADS)),
        'ssd_norm_g': gain((DEPTH, BRANCH)),
        'gm_norm_g': gain((DEPTH, BRANCH)),
        'gm_norm_b': nrm((DEPTH, BRANCH), 0.02),
        'gm_ws': nrm((DEPTH, GM_HEADS, GM_CHUNK, GM_CHUNK), GM_CHUNK ** -0.5),
        'gm_bs': gain((DEPTH, GM_HEADS, GM_CHUNK)),
        'sc_conv_w': nrm((DEPTH, SC_CONV, BRANCH), SC_CONV ** -0.5),
        'cf_conv_w': nrm((DEPTH, CF_CONV, BRANCH), CF_CONV ** -0.5),
        'cf_conv_b': nrm((DEPTH, BRANCH), 0.02),
        'cf_norm_g': gain((DEPTH, BRANCH)),
        'cf_norm_b': nrm((DEPTH, BRANCH), 0.02),
        'ffn_w1': nrm((N_DENSE, D_MODEL, D_FF), D_MODEL ** -0.5),
        'ffn_w3': nrm((N_DENSE, D_MODEL, D_FF), D_MODEL ** -0.5),
        'ffn_w2': nrm((N_DENSE, D_FF, D_MODEL), D_FF ** -0.5),
        'moe_router': nrm((N_MOE, D_MODEL, N_EXPERTS), D_MODEL ** -0.5),
        'moe_w1': nrm((N_MOE, N_EXPERTS, D_MODEL, E_FF), D_MODEL ** -0.5),
        'moe_w3': nrm((N_MOE, N_EXPERTS, D_MODEL, E_FF), D_MODEL ** -0.5),
        'moe_w2': nrm((N_MOE, N_EXPERTS, E_FF, D_MODEL), E_FF ** -0.5),
        'final_norm_g': gain((D_MODEL,)),
    }


def reference(x, c, ctx, c_ctx, mod_w, mod_b, norm1_g, norm2_g, w_in, w_out,
              ssd_conv_w, ssd_conv_b, ssd_dt_bias, ssd_a_log, ssd_d, ssd_norm_g,
              gm_norm_g, gm_norm_b, gm_ws, gm_bs, sc_conv_w, cf_conv_w, cf_conv_b, cf_norm_g, cf_norm_b,
              ffn_w1, ffn_w3, ffn_w2, moe_router, moe_w1, moe_w3, moe_w2, final_norm_g):
    bsz = x.shape[0]
    xc = ctx
    silu_c = jax.nn.silu(c)
    silu_cc = jax.nn.silu(c_ctx)
    zero_state = jnp.zeros((bsz, SSD_HEADS, HEAD_DIM, SSD_STATE), jnp.float32)
    for l in range(DEPTH):
        last = l == DEPTH - 1
        p = {'ssd_conv_w': ssd_conv_w[l], 'ssd_conv_b': ssd_conv_b[l], 'ssd_dt_bias': ssd_dt_bias[l],
             'ssd_a_log': ssd_a_log[l], 'ssd_d': ssd_d[l], 'ssd_norm_g': ssd_norm_g[l],
             'gm_norm_g': gm_norm_g[l], 'gm_norm_b': gm_norm_b[l], 'gm_ws': gm_ws[l], 'gm_bs': gm_bs[l],
             'sc_conv_w': sc_conv_w[l], 'cf_conv_w': cf_conv_w[l], 'cf_conv_b': cf_conv_b[l],
             'cf_norm_g': cf_norm_g[l], 'cf_norm_b': cf_norm_b[l]}
        sh1, sc1, g1, sh2, sc2, g2 = jnp.split((silu_c @ mod_w[l] + mod_b[l])[:, None, :], 6, axis=-1)
        csh1, csc1, cg1, csh2, csc2, cg2 = jnp.split(silu_cc @ mod_w[l] + mod_b[l], 6, axis=-1)
        h = modulate(rmsnorm(x, norm1_g[l]), sh1, sc1)
        hc = modulate(rmsnorm(xc, norm1_g[l]), csh1, csc1)
        if last:
            st_f, st_b = ssd_context_states(hc @ w_in[l][:, :SSD_CTX_COLS], p, zero_state)
        else:
            mix_c, st_f, st_b = mixer_branches(hc @ w_in[l], p, zero_state, zero_state)
            xc = xc + cg1 * (mix_c @ w_out[l])
        mix, _, _ = mixer_branches(h @ w_in[l], p, st_f, st_b)
        x = x + g1 * (mix @ w_out[l])
        h2 = modulate(rmsnorm(x, norm2_g[l]), sh2, sc2)
        x = x + g2 * channel_mixer(h2, l, ffn_w1, ffn_w3, ffn_w2, moe_router, moe_w1, moe_w3, moe_w2)
        if not last:
            hc2 = modulate(rmsnorm(xc, norm2_g[l]), csh2, csc2)
            xc = xc + cg2 * channel_mixer(hc2, l, ffn_w1, ffn_w3, ffn_w2, moe_router, moe_w1, moe_w3, moe_w2)
    return rmsnorm(x, final_norm_g)
```

```python
import numpy as np
from contextlib import ExitStack
import concourse.bass as bass
import concourse.mybir as mybir
from concourse.bass_utils import run_bass_kernel_spmd

F32 = mybir.dt.float32
BF16 = mybir.dt.bfloat16
AF = mybir.ActivationFunctionType
ALU = mybir.AluOpType
AX = mybir.AxisListType

L = 2048
CT = 256
LT = L + CT
D = 1024
NCH = LT // 128
INC = 2824
DFF = 2816
EFF = 3584
NE = 8
TT_MAIN = [(0, 512), (512, 512), (1024, 512), (1536, 512)]
TT_ALL = TT_MAIN + [(2048, 256)]
PADC = 15
RAWW = PADC + L + PADC + CT + PADC


def rawcol(t0):
    return t0 + PADC if t0 < L else t0 + 2 * PADC


def _pp_layout():
    off = {}
    n = 0
    for l in range(2):
        for name, w in (("n1g", 8), ("n2g", 8), ("scw", 30), ("scb", 6), ("sD", 2), ("sng", 2),
                        ("ccw", 6), ("fcw", 62), ("fcb", 2), ("fng", 2), ("fnb", 2), ("gbs", 4), ("modb", 48)):
            off[(name, l)] = n
            n += w
    off[("fin", 0)] = n
    n += 8
    return off, n


PPO, NPP = _pp_layout()
RBW = 8 + 8 + 256 + 256
C_ID, C_TU, C_TL, C_MF, C_MB, C_ON, C_S4, C_S8 = 0, 128, 256, 384, 512, 640, 768, 1280
NCST = 1280 + 1024


import os as _os
FORCE_INC = bool(_os.environ.get('NOINC'))


class Sched:
    EPOCH = 28000

    def __init__(self, nc, self_sync=False):
        self.nc = nc
        self.eng = {'pe': nc.tensor, 'act': nc.scalar, 'dve': nc.vector, 'pool': nc.gpsimd, 'sp': nc.sync}
        self.semobj = {}
        self.cur = {}
        self.cnt = {}
        self.nid = 0
        self.self_sync = self_sync
        for e in self.eng:
            self._newsem(e)
        self.seen = {e: {} for e in self.eng}
        self.res = {}
        self.slots = []
        self.nwait = 0

    def _newsem(self, e):
        self.nid += 1
        key = (e, self.nid)
        self.semobj[key] = self.nc.alloc_semaphore(f"s_{e}_{self.nid}")
        self.cur[e] = key
        self.cnt[e] = 0

    def slot(self, name):
        self.nid += 1
        key = ('dma', self.nid)
        self.semobj[key] = self.nc.alloc_semaphore(f"d_{name}_{self.nid}")
        s = {'key': key, 'cnt': 0}
        self.slots.append(s)
        return s

    def _wait(self, e, toks):
        need = {}
        for t in toks:
            if t is None:
                continue
            k, v = t
            if k[0] == e and not (self.self_sync and e in ('act', 'dve')):
                continue
            if self.seen[e].get(k, 0) >= v:
                continue
            if need.get(k, 0) < v:
                need[k] = v
        for k, v in need.items():
            self.eng[e].wait_ge(self.semobj[k], v)
            self.seen[e][k] = v
            self.nwait += 1

    def _deps(self, r, w):
        toks = []
        for key in r:
            ent = self.res.get(key)
            if ent is not None and ent[0] is not None:
                toks.append(ent[0])
        for key in w:
            ent = self.res.get(key)
            if ent is not None:
                if ent[0] is not None:
                    toks.append(ent[0])
                toks.extend(ent[1].items())
        return toks

    def _record(self, tok, r, w):
        for k2 in w:
            self.res[k2] = [tok, {}]
        for k2 in r:
            ent = self.res.get(k2)
            if ent is None:
                ent = self.res[k2] = [None, {}]
            if ent[1].get(tok[0], 0) < tok[1]:
                ent[1][tok[0]] = tok[1]

    def op(self, e, fn, r=(), w=(), inc=True):
        self.nops = getattr(self, 'nops', 0) + 1
        if getattr(self, 'limit', None) is not None and self.nops > self.limit:
            raise StopIteration
        pr = [k for k in r if isinstance(k, tuple) and k[0] in ('ps', 'psb')]
        if pr:
            r = [k for k in r if k not in pr]
            w = list(w) + pr
        self._wait(e, self._deps(r, w))
        ins = fn(self.eng[e])
        key = self.cur[e]
        if not inc and not FORCE_INC:
            tok = (key, self.cnt[e] + 1)
            self._record(tok, r, w)
            return tok
        ins.then_inc(self.semobj[key], 1)
        self.cnt[e] += 1
        tok = (key, self.cnt[e])
        self._record(tok, r, w)
        if self.cnt[e] >= self.EPOCH:
            self._newsem(e)
        return tok

    def dma(self, q, slot, items, r=(), w=()):
        self._wait(q, self._deps(r, w))
        for (o, i) in items:
            self.eng[q].dma_start(out=o, in_=i).then_inc(self.semobj[slot['key']], 16)
            slot['cnt'] += 16
        tok = (slot['key'], slot['cnt'])
        self._record(tok, r, w)
        return tok

    def barrier(self):
        toks = [(self.cur[e], self.cnt[e]) for e in self.eng if self.cnt[e] > 0]
        toks += [(s['key'], s['cnt']) for s in self.slots if s['cnt'] > 0]
        for e in self.eng:
            self._wait(e, toks)
        self.res = {}


def build(nseq=2, debug=None, self_sync=True):
    nc = bass.Bass("TRN2", target_bir_lowering=False)
    S = Sched(nc, self_sync=self_sync)
    es = ExitStack()

    SHAPES = {"xT": [nseq, D, L], "cxT": [nseq, D, CT], "cT": [128, 8, 3], "pp": [128, NPP], "rb": [128, 2 * RBW],
              "cst": [128, NCST], "rt": [128, 8, 8], "wsT": [128, 2 * 4 * 128], "mod_w": [2, D, 6 * D],
              "w_in": [2, D, INC], "w_out": [2, D, D], "ffn_w1": [1, D, DFF], "ffn_w3": [1, D, DFF],
              "ffn_w2": [1, DFF, D], "moe_w1": [1, NE, D, EFF], "moe_w3": [1, NE, D, EFF], "moe_w2": [1, NE, EFF, D]}
    DECL = {}

    def dd(name):
        if name not in DECL:
            DECL[name] = nc.dram_tensor(name, SHAPES[name], F32, kind="ExternalInput").ap()
        return DECL[name]

    xT, cxT, cTd, ppd, rbd, cstd, rtd, wsTd, mod_w = (dd(n_) for n_ in
                                                      ("xT", "cxT", "cT", "pp", "rb", "cst", "rt", "wsT", "mod_w"))
    outT = nc.dram_tensor("outT", [nseq, D, L], F32, kind="ExternalOutput").ap()
    dbg = None
    if debug is not None:
        dbg = nc.dram_tensor("dbg", [128, 8 * LT], F32, kind="ExternalOutput").ap()

    uid = [0]

    def sb(name, shape, dt, stack=es):
        uid[0] += 1
        return stack.enter_context(nc.sbuf_tensor(f"{name}_{uid[0]}", shape, dt))

    X = sb("X", [128, 8, LT], F32)
    H = sb("H", [128, 8, LT], BF16)
    PP = sb("PP", [128, NPP], F32)
    RB = sb("RB", [128, 2 * RBW], F32)
    CST = sb("CST", [128, NCST], F32)
    RT = sb("RT", [128, 8, 8], F32)
    IDB = sb("IDB", [128, 128], BF16)
    ONESB = sb("ONESB", [128, 128], BF16)
    WST = sb("WSTb", [128, 2 * 4 * 128], BF16)
    CTs = sb("CTs", [128, 8, 3], F32)
    SCb = sb("SCb", [128, 8, 3], BF16)
    MOD = sb("MOD", [128, 2, 48, 3], F32)
    ATAB = sb("ATAB", [128, 2, 2, 8, 3], F32)
    NEGA = sb("NEGA", [128, 2, 8], F32)
    EPS6 = sb("EPS6", [128, 1], F32)
    EPS5 = sb("EPS5", [128, 1], F32)

    PS = [es.enter_context(nc.psum_tensor(f"ps{i}", [128, 512], F32)) for i in range(6)]
    PSB = [es.enter_context(nc.psum_tensor(f"psb{i}", [128, 1024], BF16)) for i in range(2)]
    st = {'ps': 0, 'psb': 0}

    def psum():
        i = st['ps']
        st['ps'] = (i + 1) % 6
        return PS[i], ('ps', i)

    def psumb():
        i = st['psb']
        st['psb'] = (i + 1) % 2
        return PSB[i], ('psb', i)

    sl_init = S.slot("init")
    sl_x = S.slot("x")
    sl_out = [S.slot("out0"), S.slot("out1")]
    sl_dbg = S.slot("dbg")

    def ppc(name, l, i=0, n=1):
        o = PPO[(name, l)] + i
        return PP[:, o:o + n]

    S.dma('sp', sl_init, [(PP[:], ppd), (RB[:], rbd), (CST[:], cstd), (RT[:], rtd), (CTs[:], cTd)],
          w=['PP', 'RB', 'CST', 'RT', 'CTs'])
    sl_init2 = S.slot("init2")
    S.dma('pool', sl_init2, [(IDB[:], cstd[:, C_ID:C_ID + 128]), (ONESB[:], cstd[:, C_ON:C_ON + 128]),
                             (WST[:], wsTd)], w=['IDB', 'ONESB', 'WST'])
    S.op('dve', lambda e: e.memset(EPS6[:], 1e-6), w=['EPS6'])
    S.op('dve', lambda e: e.memset(EPS5[:], 1e-5), w=['EPS6'])
    S.op('act', lambda e: e.activation(out=SCb[:], in_=CTs[:], func=AF.Silu), r=['CTs'], w=['SCb'])
    for l in range(2):
        S.op('act', lambda e: e.activation(out=NEGA[:, l, :], in_=RB[:, l * RBW + 8:l * RBW + 16], func=AF.Exp),
             r=['RB'], w=[('NEGA', l)])
        S.op('dve', lambda e: e.tensor_scalar(out=NEGA[:, l, :], in0=NEGA[:, l, :], scalar1=-1.0, scalar2=0.0,
                                              op0=ALU.mult, op1=ALU.add), r=[('NEGA', l)], w=[('NEGA', l)])

    def load_x(b):
        items = []
        for k in range(8):
            items.append((X[:, k, 0:L], xT[b, k * 128:(k + 1) * 128, :]))
            items.append((X[:, k, L:LT], cxT[b, k * 128:(k + 1) * 128, :]))
        S.dma('sp', sl_x, items, w=[('X', k, tt) for k in range(8) for tt in range(5)])

    load_x(0)
    with ExitStack() as ph:
        MW = [sb(f"MW{i}", [128, 8, 512], BF16, ph) for i in range(2)]
        sl_mw = [S.slot("mw0"), S.slot("mw1")]
        n = 0
        for l in range(2):
            pm, pmk = psum()
            for ct in range(12):
                s = n % 2
                n += 1
                S.dma('pool', sl_mw[s], [(MW[s][:], mod_w[l, :, ct * 512:(ct + 1) * 512]
                                          .rearrange("(k p) n -> p k n", p=128))], w=[('MW', s)])
                for blk in range(4):
                    col = ct * 4 + blk
                    for k in range(8):
                        S.op('pe', lambda e: e.matmul(pm[:, col * 3:col * 3 + 3], MW[s][:, k, blk * 128:(blk + 1) * 128],
                                                      SCb[:, k, :], start=(k == 0), stop=(k == 7)),
                             r=[('MW', s), 'SCb'], w=[pmk])
            mb = ppc("modb", l, 0, 48)
            S.op('dve', lambda e: e.tensor_tensor(out=MOD[:, l], in0=pm[:, 0:144].rearrange("p (c j) -> p c j", j=3),
                                                  in1=mb.unsqueeze(2).to_broadcast([128, 48, 3]), op=ALU.add),
                 r=[pmk, 'PP'], w=[('MOD', l)])
            for ni, (gname, sco) in enumerate((("n1g", 8), ("n2g", 32))):
                g = ppc(gname, l, 0, 8)
                S.op('dve', lambda e: e.scalar_tensor_tensor(out=ATAB[:, l, ni], in0=MOD[:, l, sco:sco + 8, :], scalar=1.0,
                                                             in1=g.unsqueeze(2).to_broadcast([128, 8, 3]),
                                                             op0=ALU.add, op1=ALU.mult),
                     r=[('MOD', l), 'PP'], w=[('ATAB', l, ni)])
        S.barrier()

    def dump(ap_list):
        o = 0
        items = []
        for ap, n_ in ap_list:
            items.append((dbg[:, o:o + n_], ap))
            o += n_
        S.barrier()
        tok = S.dma('pool', sl_dbg, items)
        S._wait('sp', [tok])

    def norm_phase(l, ni, j, tiles, moe=None):
        sh0 = 0 if ni == 0 else 24
        with ExitStack() as ph:
            SQ = [sb(f"SQ{i}", [128, 512], BF16, ph) for i in range(4)]
            RS = [sb(f"RS{i}", [128, 512], F32, ph) for i in range(2)]
            TMP = [sb(f"TMP{i}", [128, 512], F32, ph) for i in range(4)]
            H32s = [sb(f"H32_{i}", [128, 8, 512], F32, ph) for i in range(2)] if moe is not None else None

            def norm_tile(t0, T, g):
                tt = t0 // 512
                jj = j if t0 < L else 2
                H32 = H32s[g] if moe is not None else None
                pss, pk = psum()
                for k in range(8):
                    q = 2 * g + k % 2
                    S.op('act', lambda e: e.activation(out=SQ[q][:, :T], in_=X[:, k, t0:t0 + T], func=AF.Square),
                         r=[('X', k, tt)], w=[('SQ', q)])
                    S.op('pe', lambda e: e.matmul(pss[:, :T], ONESB[:], SQ[q][:, :T], start=(k == 0), stop=(k == 7)),
                         r=[('SQ', q), 'ONESB'], w=[pk])
                    if k % 2 == 1:
                        yield
                rq = g
                S.op('act', lambda e: e.activation(out=RS[rq][:, :T], in_=pss[:, :T], func=AF.Sqrt, bias=EPS6[:, 0:1],
                                                   scale=1.0 / D), r=[pk, 'EPS6'], w=[('RS', rq)])
                yield
                S.op('dve', lambda e: e.reciprocal(out=RS[rq][:, :T], in_=RS[rq][:, :T]), r=[('RS', rq)], w=[('RS', rq)])
                yield
                for k in range(8):
                    q = 2 * g + k % 2
                    S.op('dve', lambda e: e.tensor_tensor(out=TMP[q][:, :T], in0=X[:, k, t0:t0 + T], in1=RS[rq][:, :T],
                                                          op=ALU.mult), r=[('X', k, tt), ('RS', rq)], w=[('TMP', q)])
                    a_ap = ATAB[:, l, ni, k, jj:jj + 1]
                    b_ap = MOD[:, l, sh0 + k, jj:jj + 1]
                    if moe is None:
                        S.op('act', lambda e: e.activation(out=H[:, k, t0:t0 + T], in_=TMP[q][:, :T], func=AF.Identity,
                                                           bias=b_ap, scale=a_ap),
                             r=[('TMP', q)], w=[('H', k, tt)])
                    else:
                        S.op('act', lambda e: e.activation(out=H32[:, k, :T], in_=TMP[q][:, :T], func=AF.Identity,
                                                           bias=b_ap, scale=a_ap),
                             r=[('TMP', q)], w=[('H32', g, k)])
                        S.op('dve', lambda e: e.tensor_copy(out=H[:, k, t0:t0 + T], in_=H32[:, k, :T]),
                             r=[('H32', g, k)], w=[('H', k, tt)])
                    if k % 2 == 1:
                        yield
                if moe is not None:
                    pl, plk = moe
                    for sub in range(T // 128):
                        c0 = (t0 // 128 + sub) * 8
                        for k in range(8):
                            S.op('pe', lambda e: e.matmul(pl[:, c0:c0 + 8], H32[:, k, sub * 128:(sub + 1) * 128], RT[:, k, :],
                                                          start=(k == 0), stop=(k == 7)),
                                 r=[('H32', g, k), 'RT'], w=[plk])

            pending = [norm_tile(t0, T, i % 2) for i, (t0, T) in enumerate(tiles)]
            active = []
            while pending or active:
                if pending and len(active) < 2:
                    active.append(pending.pop(0))
                for g_ in list(active):
                    try:
                        next(g_)
                    except StopIteration:
                        active.remove(g_)
            S.barrier()


    def proj(ps_ap, W, c0, t0, T, wkey, pk, n=128):
        for k in range(8):
            S.op('pe', lambda e: e.matmul(ps_ap, W[:, k, c0:c0 + n], H[:, k, t0:t0 + T], start=(k == 0), stop=(k == 7)),
                 r=[wkey, ('H', k, t0 // 512)], w=[pk], inc=(k == 7))

    def brkeys(t0, T):
        return [('BR', c) for c in range(t0 // 128, (t0 + T) // 128)]

    def out_proj(l, br, BR, WO, sl_wo, tiles, j):
        S.dma('pool', sl_wo, [(WO[:], dd("w_out")[l, br * 256:(br + 1) * 256, :].rearrange("(k p) n -> p k n", p=128))],
              w=['WO'])
        for (t0, T) in tiles:
            tt = t0 // 512
            jj = j if t0 < L else 2
            for kb in range(8):
                po, pk = psum()
                for jb in range(2):
                    S.op('pe', lambda e: e.matmul(po[:, :T], WO[:, jb, kb * 128:(kb + 1) * 128], BR[:, jb, t0:t0 + T],
                                                  start=(jb == 0), stop=(jb == 1)), r=['WO'] + brkeys(t0, T), w=[pk], inc=(jb == 1))
                g_ap = MOD[:, l, 16 + kb, jj:jj + 1]
                S.op('dve', lambda e: e.scalar_tensor_tensor(out=X[:, kb, t0:t0 + T], in0=po[:, :T], scalar=g_ap,
                                                             in1=X[:, kb, t0:t0 + T], op0=ALU.mult, op1=ALU.add),
                     r=[pk, ('X', kb, tt)], w=[('X', kb, tt)])

    def out_proj_multi(l, parts, tiles, j):
        for (br, BRx, WOx, wokey, brname, slot) in parts:
            S.dma('pool', slot, [(WOx[:], dd("w_out")[l, br * 256:(br + 1) * 256, :].rearrange("(k p) n -> p k n", p=128))],
                  w=[wokey])
        n = 2 * len(parts)
        for (t0, T) in tiles:
            tt = t0 // 512
            jj = j if t0 < L else 2
            for kb in range(8):
                po, pk = psum()
                idx = 0
                for (br, BRx, WOx, wokey, brname, slot) in parts:
                    for jb in range(2):
                        S.op('pe', lambda e: e.matmul(po[:, :T], WOx[:, jb, kb * 128:(kb + 1) * 128], BRx[:, jb, t0:t0 + T],
                                                      start=(idx == 0), stop=(idx == n - 1)),
                             r=[wokey] + [(brname, c_) for c_ in range(t0 // 128, (t0 + T) // 128)], w=[pk], inc=(idx == n - 1))
                        idx += 1
                g_ap = MOD[:, l, 16 + kb, jj:jj + 1]
                S.op('dve', lambda e: e.scalar_tensor_tensor(out=X[:, kb, t0:t0 + T], in0=po[:, :T], scalar=g_ap,
                                                             in1=X[:, kb, t0:t0 + T], op0=ALU.mult, op1=ALU.add),
                     r=[pk, ('X', kb, tt)], w=[('X', kb, tt)])

    def build_diag(DG, l, name, blk, ntap):
        for k in range(ntap):
            wap = ppc(name, l, blk * ntap + k)
            S.op('pool', lambda e: e.tensor_scalar(out=DG[:, k, :], in0=IDB[:], scalar1=wap, scalar2=0.0,
                                                   op0=ALU.mult, op1=ALU.add), r=['IDB', 'PP'], w=['DG'])

    def conv_mm(ps_ap, DG, RAW, rkey, t0, T, ntap, pk):
        half = ntap // 2
        c0 = rawcol(t0)
        for k in range(ntap):
            S.op('pe', lambda e: e.matmul(ps_ap, DG[:, k, :], RAW[:, c0 + k - half:c0 + k - half + T],
                                          start=(k == 0), stop=(k == ntap - 1)), r=['DG', rkey], w=[pk], inc=(k == ntap - 1))

    def mixer_phase(l, j, last):
        tiles_out = TT_MAIN if last else TT_ALL
        win = dd("w_in")
        with ExitStack() as ph:
            WO = sb("WO", [128, 2, 1024], BF16, ph)
            BR = sb("BR", [128, 2, LT], BF16, ph)
            sl_wo = S.slot("wo")
            sl_wm = S.slot("wm")
            sl_wm2 = S.slot("wm2")

            with ExitStack() as pa:
                WMb = sb("WMb", [128, 8, 264], BF16, pa)
                XBC = sb("XBC", [128, 6, LT], BF16, pa)
                S.dma('pool', sl_wm2, [(WMb[:], win[l, :, 768:1032].rearrange("(k p) n -> p k n", p=128))], w=['WMb'])
                with ExitStack() as p1:
                    WMa = sb("WMa", [128, 8, 768], BF16, p1)
                    RAW = [sb(f"RAW{i}", [128, RAWW], BF16, p1) for i in range(2)]
                    DG = sb("DG", [128, 5, 128], BF16, p1)
                    S.dma('pool', sl_wm, [(WMa[:], win[l, :, 0:768].rearrange("(k p) n -> p k n", p=128))], w=['WMa'])
                    for i in range(2):
                        S.op('dve', lambda e: e.memset(RAW[i][:], 0.0), w=[('RAW', i)])
                    for blk in range(6):
                        rs = blk % 2
                        for (t0, T) in TT_ALL:
                            ps, pk = psum()
                            proj(ps[:, :T], WMa, blk * 128, t0, T, 'WMa', pk)
                            S.op('act', lambda e: e.activation(out=RAW[rs][:, rawcol(t0):rawcol(t0) + T], in_=ps[:, :T],
                                                               func=AF.Copy), r=[pk], w=[('RAW', rs)])
                        build_diag(DG, l, "scw", blk, 5)
                        for (t0, T) in TT_ALL:
                            ps, pk = psum()
                            conv_mm(ps[:, :T], DG, RAW[rs], ('RAW', rs), t0, T, 5, pk)
                            S.op('act', lambda e: e.activation(out=XBC[:, blk, t0:t0 + T], in_=ps[:, :T], func=AF.Silu,
                                                               bias=ppc("scb", l, blk), scale=1.0),
                                 r=[pk, 'PP'], w=[('XBC', blk, t0 // 512)])
                    S.barrier()
                if debug == 'xbc' and l == 0:
                    dump([(XBC[:, g, :], LT) for g in range(6)])
                    return 'stop'
                sm = {n_: sb("sm_" + n_, [128, NCH, 2, 4], F32, pa) for n_ in
                      ("DT", "DTA", "ACS", "TOTS", "DTE", "CD", "W2")}
                fl = lambda t: t[:].rearrange("p c d h -> p (c d h)")
                pd, pdk = psum()
                for c in range(NCH):
                    for k in range(8):
                        S.op('pe', lambda e: e.matmul(pd[:, c * 8:(c + 1) * 8], H[:, k, c * 128:(c + 1) * 128], WMb[:, k, 0:8],
                                                      start=(k == 0), stop=(k == 7)), r=['WMb', ('H', k, c // 4)], w=[pdk])
                dtb = RB[:, l * RBW:l * RBW + 8]
                S.op('dve', lambda e: e.tensor_tensor(out=sm["DT"][:].rearrange("p c d h -> p c (d h)"),
                                                      in0=pd[:, 0:NCH * 8].rearrange("p (c x) -> p c x", x=8),
                                                      in1=dtb.unsqueeze(1).to_broadcast([128, NCH, 8]), op=ALU.add),
                     r=[pdk, 'RB'], w=['DT'])
                S.op('act', lambda e: e.activation(out=fl(sm["DT"]), in_=fl(sm["DT"]), func=AF.Exp), r=['DT'], w=['DT'])
                S.op('act', lambda e: e.activation(out=fl(sm["DT"]), in_=fl(sm["DT"]), func=AF.Ln, bias=1.0, scale=1.0),
                     r=['DT'], w=['DT'])
                S.op('dve', lambda e: e.tensor_tensor(out=sm["DTA"][:].rearrange("p c d h -> p c (d h)"),
                                                      in0=sm["DT"][:].rearrange("p c d h -> p c (d h)"),
                                                      in1=NEGA[:, l, :].unsqueeze(1).to_broadcast([128, NCH, 8]), op=ALU.mult),
                     r=['DT', ('NEGA', l)], w=['DTA'])
                pf, pfk = psum()
                pb_, pbk = psum()
                pt, ptk = psum()
                n8 = NCH * 8
                S.op('pe', lambda e: e.matmul(pf[:, :n8], CST[:, C_TU:C_TU + 128], fl(sm["DTA"]), start=True, stop=True),
                     r=['DTA', 'CST'], w=[pfk])
                S.op('pe', lambda e: e.matmul(pb_[:, :n8], CST[:, C_TL:C_TL + 128], fl(sm["DTA"]), start=True, stop=True),
                     r=['DTA', 'CST'], w=[pbk])
                S.op('pe', lambda e: e.matmul(pt[:, :n8], CST[:, C_ON:C_ON + 128], fl(sm["DTA"]), start=True, stop=True),
                     r=['DTA', 'CST'], w=[ptk])
                v4 = lambda p_: p_[:, :n8].rearrange("p (c d h) -> p c d h", d=2, h=4)
                S.op('dve', lambda e: e.tensor_copy(out=sm["ACS"][:, :, 0, :], in_=v4(pf)[:, :, 0, :]), r=[pfk], w=['ACS'])
                S.op('dve', lambda e: e.tensor_copy(out=sm["ACS"][:, :, 1, :], in_=v4(pb_)[:, :, 1, :]), r=[pbk], w=['ACS'])
                S.op('dve', lambda e: e.tensor_copy(out=fl(sm["TOTS"]), in_=pt[:, :n8]), r=[ptk], w=['TOTS'])
                S.op('dve', lambda e: e.tensor_tensor(out=fl(sm["DTE"]), in0=fl(sm["TOTS"]), in1=fl(sm["ACS"]),
                                                      op=ALU.subtract), r=['TOTS', 'ACS'], w=['DTE'])
                S.op('act', lambda e: e.activation(out=fl(sm["DTE"]), in_=fl(sm["DTE"]), func=AF.Exp), r=['DTE'], w=['DTE'])
                S.op('act', lambda e: e.activation(out=fl(sm["CD"]), in_=fl(sm["TOTS"]), func=AF.Exp), r=['TOTS'], w=['CD'])
                S.op('dve', lambda e: e.tensor_tensor(out=fl(sm["W2"]), in0=fl(sm["DT"]), in1=fl(sm["DTE"]), op=ALU.mult),
                     r=['DT', 'DTE'], w=['W2'])
                if debug == 'dt' and l == 0:
                    dump([(fl(sm[n_]), NCH * 8) for n_ in ("DT", "DTA", "ACS", "TOTS", "DTE", "CD", "W2")])
                    return 'stop'
                XDTM = [[sb(f"XDTM{d}{hh}", [128, 2, 128], BF16, pa) for hh in range(2)] for d in range(2)]
                ENTM = [[sb(f"ENTM{d}{hh}", [128, 2, 128], BF16, pa) for hh in range(2)] for d in range(2)]
                STATE = [sb(f"STATE{d}", [128, 256], F32, pa) for d in range(2)]
                XDTD = [sb(f"XDTD{d}", [128, 256], BF16, pa) for d in range(2)]
                BTOK = [sb(f"BTOK{d}", [128, 256], BF16, pa) for d in range(2)]
                ACSTc = [sb(f"ACSTc{d}", [4, 128], F32, pa) for d in range(2)]
                DM = [sb(f"DM{d}", [128, 4, 128], F32, pa) for d in range(2)]
                LM = [sb(f"LM{d}", [128, 4, 128], BF16, pa) for d in range(2)]
                EB = [sb(f"EB{d}", [128, 4, 128], BF16, pa) for d in range(2)]
                MT = [sb(f"MT{d}", [128, 4, 128], BF16, pa) for d in range(2)]
                CE = [sb(f"CE{d}", [128, 4, 128], BF16, pa) for d in range(2)]
                YT_ = [sb(f"YT{d}", [128, 2, 128], F32, pa) for d in range(2)]
                ZS_ = [sb(f"ZS{d}", [128, 2, 128], F32, pa) for d in range(2)]
                SQy_ = [sb(f"SQy{d}", [128, 2, 128], BF16, pa) for d in range(2)]
                RSy_ = [sb(f"RSy{d}", [128, 128], F32, pa) for d in range(2)]
                for d in range(2):
                    for hh in range(2):
                        S.op('dve', lambda e: e.memset(XDTM[d][hh][:], 0.0), w=[('XDTM', d, hh)])
                        S.op('dve', lambda e: e.memset(ENTM[d][hh][:], 0.0), w=[('ENTM', d, hh)])
                    S.op('dve', lambda e: e.memset(STATE[d][:], 0.0), w=[('STATE', d)])
                orders = [[16, 17] + list(range(16)), [17, 16] + list(range(15, -1, -1))]
                pos = [{c: i for i, c in enumerate(orders[d])} for d in range(2)]

                def chunk_step(d, c):
                    tri = CST[:, C_TU:C_TU + 128] if d == 0 else CST[:, C_TL:C_TL + 128]
                    mneg = CST[:, C_MF:C_MF + 128] if d == 0 else CST[:, C_MB:C_MB + 128]
                    bA, bAk = PS[3 * d], ('ps', 3 * d)
                    bB, bBk = PS[3 * d + 1], ('ps', 3 * d + 1)
                    bC, bCk = PS[3 * d + 2], ('ps', 3 * d + 2)
                    pT, pTk = PSB[d], ('psb', d)
                    cs = slice(c * 128, (c + 1) * 128)
                    tt = c // 4
                    want_y = (c < 16) or (not last)
                    final = pos[d][c] > pos[1 - d][c]
                    YT, ZS, SQy, RSy = YT_[d], ZS_[d], SQy_[d], RSy_[d]
                    kYT, kZS, kSQ, kRS = ('YT', d), ('ZS', d), ('SQy', d), ('RSy', d)
                    for i4 in range(4):
                        S.op('pe', lambda e: e.transpose(pT[:, i4 * 128:(i4 + 1) * 128], XBC[:, i4, cs], IDB[:]),
                             r=[('XBC', i4, tt), 'IDB'], w=[pTk])
                    if want_y:
                        S.op('pe', lambda e: e.matmul(bB[0:4, 0:128], sm["DTA"][:, c, d, :], tri, start=True, stop=True),
                             r=['DTA', 'CST'], w=[bBk])
                    yield
                    w2 = sm["W2"][:, c, d, :]
                    S.op('dve', lambda e: e.tensor_tensor(out=XDTD[d][:].rearrange("p (h x) -> p h x", x=64),
                                                          in0=pT[:, 0:256].rearrange("p (h x) -> p h x", x=64),
                                                          in1=w2.unsqueeze(2).to_broadcast([128, 4, 64]), op=ALU.mult),
                         r=[pTk, 'W2'], w=[('XDTD', d)])
                    S.op('act', lambda e: e.activation(out=BTOK[d][:], in_=pT[:, 256:512], func=AF.Copy), r=[pTk], w=[('BTOK', d)])
                    if want_y:
                        S.op('act', lambda e: e.activation(out=ACSTc[d][:], in_=bB[0:4, 0:128], func=AF.Copy),
                             r=[bBk], w=[('ACSTc', d)])
                        w1 = sm["DT"][:, c, d, :].rearrange("p (g hh) -> p g hh", hh=2)
                        for hh in range(2):
                            S.op('dve', lambda e: e.tensor_tensor(
                                out=XDTM[d][hh][:, :, hh * 64:(hh + 1) * 64],
                                in0=pT[:, 0:256].rearrange("p (g hh x) -> p g hh x", g=2, hh=2)[:, :, hh, :],
                                in1=w1[:, :, hh].unsqueeze(2).to_broadcast([128, 2, 64]), op=ALU.mult),
                                r=[pTk, 'DT'], w=[('XDTM', d, hh)])
                    yield
                    for g in range(2):
                        S.op('pe', lambda e: e.matmul(bA[:, g * 128:(g + 1) * 128], BTOK[d][:, g * 128:(g + 1) * 128],
                                                      XDTD[d][:, g * 128:(g + 1) * 128], start=True, stop=True),
                             r=[('BTOK', d), ('XDTD', d)], w=[bAk])
                    if want_y:
                        for g in range(2):
                            S.op('pe', lambda e: e.matmul(bA[:, 256 + g * 128:256 + (g + 1) * 128], XBC[:, 2 + g, cs],
                                                          XBC[:, 4 + g, cs], start=True, stop=True),
                                 r=[('XBC', 2 + g, tt), ('XBC', 4 + g, tt)], w=[bAk])
                        for h in range(4):
                            S.op('pe', lambda e: e.matmul(bB[:, h * 128:(h + 1) * 128],
                                                          CST[0:4, C_S4 + h * 128:C_S4 + (h + 1) * 128], ACSTc[d][:],
                                                          start=True, stop=True), r=[('ACSTc', d), 'CST'], w=[bBk])
                        yield
                        pA3 = bB[:].rearrange("p (h x) -> p h x", x=128)
                        acs_c = sm["ACS"][:, c, d, :]
                        S.op('dve', lambda e: e.tensor_tensor(out=DM[d][:], in0=pA3,
                                                              in1=acs_c.unsqueeze(2).to_broadcast([128, 4, 128]),
                                                              op=ALU.subtract), r=[bBk, 'ACS'], w=[('DM', d)])
                        S.op('act', lambda e: e.activation(out=EB[d][:], in_=pA3, func=AF.Exp), r=[bBk], w=[('EB', d)])
                        S.op('dve', lambda e: e.tensor_tensor(out=DM[d][:], in0=DM[d][:],
                                                              in1=mneg.unsqueeze(1).to_broadcast([128, 4, 128]),
                                                              op=ALU.add), r=[('DM', d), 'CST'], w=[('DM', d)])
                        yield
                        S.op('act', lambda e: e.activation(out=LM[d][:], in_=DM[d][:], func=AF.Exp), r=[('DM', d)], w=[('LM', d)])
                        for g in range(2):
                            S.op('dve', lambda e: e.tensor_tensor(
                                out=CE[d][:, 2 * g:2 * g + 2, :], in0=EB[d][:, 2 * g:2 * g + 2, :],
                                in1=XBC[:, 4 + g, cs].unsqueeze(1).to_broadcast([128, 2, 128]),
                                op=ALU.mult), r=[('EB', d), ('XBC', 4 + g, tt)], w=[('CE', d)])
                        yield
                        for g in range(2):
                            S.op('dve', lambda e: e.tensor_tensor(
                                out=MT[d][:, 2 * g:2 * g + 2, :], in0=LM[d][:, 2 * g:2 * g + 2, :],
                                in1=bA[:, 256 + g * 128:256 + (g + 1) * 128].unsqueeze(1).to_broadcast([128, 2, 128]),
                                op=ALU.mult), r=[('LM', d), bAk], w=[('MT', d)])
                        yield
                        for g in range(2):
                            ops = [(XDTM[d][0][:, g, :], MT[d][:, 2 * g, :]), (XDTM[d][1][:, g, :], MT[d][:, 2 * g + 1, :]),
                                   (ENTM[d][0][:, g, :], CE[d][:, 2 * g, :]), (ENTM[d][1][:, g, :], CE[d][:, 2 * g + 1, :])]
                            for oi, (lt_, rh_) in enumerate(ops):
                                S.op('pe', lambda e: e.matmul(bC[:, g * 128:(g + 1) * 128], lt_, rh_,
                                                              start=(oi == 0), stop=(oi == 3)),
                                     r=[('XDTM', d, 0), ('XDTM', d, 1), ('ENTM', d, 0), ('ENTM', d, 1), ('MT', d), ('CE', d)],
                                     w=[bCk])
                        if final:
                            for g in range(2):
                                proj(bC[:, 256 + g * 128:256 + (g + 1) * 128], WMb, 8 + g * 128, c * 128, 128, 'WMb', bCk)
                        yield
                    cd = sm["CD"][:, c, d, :]
                    st3 = STATE[d][:].rearrange("p (h x) -> p h x", x=64)
                    S.op('dve', lambda e: e.tensor_tensor(out=st3, in0=st3, in1=cd.unsqueeze(2).to_broadcast([128, 4, 64]),
                                                          op=ALU.mult), r=[('STATE', d), 'CD'], w=[('STATE', d)])
                    S.op('dve', lambda e: e.tensor_tensor(out=STATE[d][:], in0=STATE[d][:], in1=bA[:, 0:256], op=ALU.add),
                         r=[('STATE', d), bAk], w=[('STATE', d)])
                    yield
                    for hh in range(2):
                        S.op('act', lambda e: e.activation(
                            out=ENTM[d][hh][:, :, hh * 64:(hh + 1) * 64],
                            in_=STATE[d][:].rearrange("p (g hh x) -> p g hh x", g=2, hh=2)[:, :, hh, :], func=AF.Copy),
                            r=[('STATE', d)], w=[('ENTM', d, hh)])
                    if want_y:
                        pY3 = bC[:, 0:256].rearrange("p (g x) -> p g x", x=128)
                        if not final:
                            S.op('act', lambda e: e.activation(out=BR[:, :, cs], in_=pY3, func=AF.Copy),
                                 r=[bCk], w=[('BR', c)])
                        else:
                            S.op('dve', lambda e: e.tensor_tensor(out=YT[:], in0=pY3, in1=BR[:, :, cs], op=ALU.add),
                                 r=[bCk, ('BR', c)], w=[kYT])
                            S.op('act', lambda e: e.activation(out=ZS[:], in_=bC[:, 256:512].rearrange("p (g x) -> p g x", x=128),
                                                               func=AF.Silu), r=[bCk], w=[kZS])
                            yield
                            for g in range(2):
                                S.op('dve', lambda e: e.scalar_tensor_tensor(
                                    out=YT[:, g, :], in0=XBC[:, g, cs], scalar=ppc("sD", l, g), in1=YT[:, g, :],
                                    op0=ALU.mult, op1=ALU.add), r=[('XBC', g, tt), kYT, 'PP'], w=[kYT])
                            S.op('dve', lambda e: e.tensor_tensor(out=YT[:], in0=YT[:], in1=ZS[:], op=ALU.mult),
                                 r=[kYT, kZS], w=[kYT])
                            yield
                            S.op('act', lambda e: e.activation(out=SQy[:], in_=YT[:], func=AF.Square), r=[kYT], w=[kSQ])
                            yield
                            for g in range(2):
                                S.op('pe', lambda e: e.matmul(bB[:, 0:128], ONESB[:], SQy[:, g, :], start=(g == 0), stop=(g == 1)),
                                     r=[kSQ, 'ONESB'], w=[bBk])
                            yield
                            S.op('act', lambda e: e.activation(out=RSy[:], in_=bB[:, 0:128], func=AF.Sqrt, bias=EPS6[:, 0:1],
                                                               scale=1.0 / 256), r=[bBk, 'EPS6'], w=[kRS])
                            yield
                            S.op('dve', lambda e: e.reciprocal(out=RSy[:], in_=RSy[:]), r=[kRS], w=[kRS])
                            for g in range(2):
                                S.op('dve', lambda e: e.scalar_tensor_tensor(
                                    out=BR[:, g, cs], in0=YT[:, g, :], scalar=ppc("sng", l, g), in1=RSy[:],
                                    op0=ALU.mult, op1=ALU.mult), r=[kYT, kRS, 'PP'], w=[('BR', c)])

                for i in range(NCH):
                    active = [chunk_step(0, orders[0][i]), chunk_step(1, orders[1][i])]
                    import os
                    if os.environ.get("NOINTER"):
                        for g_ in active:
                            for _ in g_:
                                pass
                        active = []
                    while active:
                        for g_ in list(active):
                            try:
                                next(g_)
                            except StopIteration:
                                active.remove(g_)
                S.barrier()
            if debug == 'ssd' and l == 0:
                dump([(BR[:, g, :], LT) for g in range(2)])
                return 'stop'
            out_chunks = list(range(16)) + ([] if last else [16, 17])
            with ExitStack() as pb:
                WMg = sb("WMg", [128, 8, 512], BF16, pb)
                U = sb("U", [128, 2, LT], F32, pb)
                VG = sb("VG", [128, 256], F32, pb)
                VG2 = sb("VG2", [128, 256], F32, pb)
                BST = sb("BST", [128, 6], F32, pb)
                MV = sb("MV", [128, 2], F32, pb)
                RSg = sb("RSg", [128, 1], F32, pb)
                VB = sb("VB", [128, 256], BF16, pb)
                SBb = sb("SBb", [128, 256], BF16, pb)
                BR2 = sb("BR2", [128, 2, LT], BF16, pb)
                WO2 = sb("WO2", [128, 2, 1024], BF16, pb)
                sl_wo2 = S.slot("wo2")
                S.dma('pool', sl_wm, [(WMg[:], win[l, :, 1032:1544].rearrange("(k p) n -> p k n", p=128))], w=['WMg'])
                for blk in range(2):
                    for (t0, T) in tiles_out:
                        ps, pk = psum()
                        proj(ps[:, :T], WMg, blk * 128, t0, T, 'WMg', pk)
                        S.op('act', lambda e: e.activation(out=U[:, blk, t0:t0 + T], in_=ps[:, :T], func=AF.Gelu_apprx_tanh),
                             r=[pk], w=[('U', t0 // 512)])
                gng = RB[:, l * RBW + 16:l * RBW + 272]
                gnb = RB[:, l * RBW + 272:l * RBW + 528]
                NG = 6
                VGs = [VG, VG2] + [sb(f"VGx{i}", [128, 256], F32, pb) for i in range(2 * NG - 2)]
                VBs = [VB] + [sb(f"VBx{i}", [128, 256], BF16, pb) for i in range(NG - 1)]
                SBs = [SBb] + [sb(f"SBx{i}", [128, 256], BF16, pb) for i in range(NG - 1)]
                MVs = [MV] + [sb(f"MVx{i}", [128, 2], F32, pb) for i in range(NG - 1)]
                RSs = [RSg] + [sb(f"RSx{i}", [128, 1], F32, pb) for i in range(NG - 1)]

                def gm_chunk(c, q):
                    VG_, VG2_, VB_, SB_, MV_, RS_ = VGs[2 * q], VGs[2 * q + 1], VBs[q], SBs[q], MVs[q], RSs[q]
                    kv, kv2, kvb, ksb, kmv, kmv2, krs = [(n_, q) for n_ in ('VG', 'VG2', 'VB', 'SBb', 'MV', 'MV2', 'RSg')]
                    cs = slice(c * 128, (c + 1) * 128)
                    pv, pvk = psum()
                    for k in range(8):
                        S.op('pe', lambda e: e.matmul(pv[:, 0:256], H[:, k, cs], WMg[:, k, 256:512], start=(k == 0), stop=(k == 7)),
                             r=['WMg', ('H', k, c // 4)], w=[pvk])
                    yield
                    S.op('act', lambda e: e.activation(out=VG_[:], in_=pv[:, 0:256], func=AF.Gelu_apprx_tanh), r=[pvk], w=[kv])
                    yield
                    S.op('dve', lambda e: e.reduce_sum(out=MV_[:, 0:1], in_=VG_[:], axis=AX.X), r=[kv], w=[kmv])
                    yield
                    S.op('dve', lambda e: e.tensor_scalar(out=MV_[:, 0:1], in0=MV_[:, 0:1], scalar1=-1.0 / 256, scalar2=0.0,
                                                          op0=ALU.mult, op1=ALU.add), r=[kmv], w=[kmv])
                    yield
                    S.op('dve', lambda e: e.tensor_scalar(out=VG_[:], in0=VG_[:], scalar1=MV_[:, 0:1], scalar2=0.0,
                                                          op0=ALU.add, op1=ALU.add), r=[kv, kmv], w=[kv])
                    yield
                    S.op('dve', lambda e: e.tensor_tensor(out=VG2_[:], in0=VG_[:], in1=VG_[:], op=ALU.mult), r=[kv], w=[kv2])
                    yield
                    S.op('dve', lambda e: e.reduce_sum(out=MV_[:, 1:2], in_=VG2_[:], axis=AX.X), r=[kv2], w=[kmv2])
                    yield
                    S.op('act', lambda e: e.activation(out=RS_[:], in_=MV_[:, 1:2], func=AF.Sqrt, bias=EPS5[:, 0:1], scale=1.0 / 256),
                         r=[kmv2, 'EPS6'], w=[krs])
                    yield
                    S.op('dve', lambda e: e.reciprocal(out=RS_[:], in_=RS_[:]), r=[krs], w=[krs])
                    yield
                    S.op('dve', lambda e: e.tensor_scalar(out=VG_[:], in0=VG_[:], scalar1=RS_[:, 0:1], scalar2=0.0,
                                                          op0=ALU.mult, op1=ALU.add), r=[kv, krs], w=[kv])
                    yield
                    S.op('dve', lambda e: e.tensor_tensor(out=VG_[:], in0=VG_[:], in1=gng, op=ALU.mult), r=[kv, 'RB'], w=[kv])
                    yield
                    S.op('dve', lambda e: e.tensor_tensor(out=VB_[:], in0=VG_[:], in1=gnb, op=ALU.add), r=[kv, 'RB'], w=[kvb])
                    yield
                    pss_, psk = psum()
                    for g in range(4):
                        S.op('pe', lambda e: e.matmul(pss_[:, g * 64:(g + 1) * 64], WST[:, (l * 4 + g) * 128:(l * 4 + g + 1) * 128],
                                                      VB_[:, g * 64:(g + 1) * 64], start=True, stop=True), r=[kvb, 'WST'], w=[psk])
                    yield
                    S.op('dve', lambda e: e.tensor_tensor(out=SB_[:].rearrange("p (g x) -> p g x", x=64),
                                                          in0=pss_[:, 0:256].rearrange("p (g x) -> p g x", x=64),
                                                          in1=ppc("gbs", l, 0, 4).unsqueeze(2).to_broadcast([128, 4, 64]),
                                                          op=ALU.add), r=[psk, 'PP'], w=[ksb])
                    yield
                    pT, pTk = psum()
                    pTb = pT[:].bitcast(BF16)
                    for blk in range(2):
                        S.op('pe', lambda e: e.transpose(pTb[:, blk * 128:(blk + 1) * 128], SB_[:, blk * 128:(blk + 1) * 128], IDB[:]),
                             r=[ksb, 'IDB'], w=[pTk])
                    yield
                    S.op('dve', lambda e: e.tensor_tensor(out=BR2[:, :, cs], in0=U[:, :, cs],
                                                          in1=pTb[:, 0:256].rearrange("p (g x) -> p g x", x=128), op=ALU.mult),
                         r=[('U', c // 4), pTk], w=[('BR2', c)])

                pend = list(out_chunks)
                free_q = list(range(NG))
                active = []
                while pend or active:
                    while pend and free_q:
                        q_ = free_q.pop(0)
                        active.append((gm_chunk(pend.pop(0), q_), q_))
                    for it in list(active):
                        try:
                            next(it[0])
                        except StopIteration:
                            active.remove(it)
                            free_q.append(it[1])
                if debug == 'gm' and l == 0:
                    dump([(BR2[:, g, :], LT) for g in range(2)])
                    return 'stop'
                out_proj_multi(l, [(0, BR, WO, 'WO', 'BR', sl_wo), (1, BR2, WO2, 'WO2', 'BR2', sl_wo2)], tiles_out, j)
                S.barrier()
            with ExitStack() as pc_:
                WMs = sb("WMs", [128, 8, 768], BF16, pc_)
                RAW = sb("RAWs", [128, RAWW], BF16, pc_)
                DG = sb("DGs", [128, 3, 128], BF16, pc_)
                TF = [sb(f"TFs{i}", [128, 512], F32, pc_) for i in range(2)]
                S.dma('pool', sl_wm, [(WMs[:], win[l, :, 1544:2312].rearrange("(k p) n -> p k n", p=128))], w=['WMs'])
                S.op('dve', lambda e: e.memset(RAW[:], 0.0), w=['RAWs'])
                nq = 0
                for blk in range(2):
                    for (t0, T) in tiles_out:
                        q = nq % 2
                        nq += 1
                        pc, pck = psum()
                        proj(pc[:, :T], WMs, 256 + blk * 128, t0, T, 'WMs', pck)
                        px, pxk = psum()
                        proj(px[:, :T], WMs, 512 + blk * 128, t0, T, 'WMs', pxk)
                        S.op('act', lambda e: e.activation(out=TF[q][:, :T], in_=pc[:, :T], func=AF.Copy), r=[pck], w=[('TFs', q)])
                        S.op('dve', lambda e: e.tensor_tensor(out=RAW[:, rawcol(t0):rawcol(t0) + T], in0=TF[q][:, :T],
                                                              in1=px[:, :T], op=ALU.mult), r=[('TFs', q), pxk], w=['RAWs'])
                    build_diag(DG, l, "ccw", blk, 3)
                    for (t0, T) in tiles_out:
                        q = nq % 2
                        nq += 1
                        pcv, pcvk = psum()
                        conv_mm(pcv[:, :T], DG, RAW, 'RAWs', t0, T, 3, pcvk)
                        pg, pgk = psum()
                        proj(pg[:, :T], WMs, blk * 128, t0, T, 'WMs', pgk)
                        S.op('act', lambda e: e.activation(out=TF[q][:, :T], in_=pcv[:, :T], func=AF.Copy), r=[pcvk], w=[('TFs', q)])
                        S.op('dve', lambda e: e.tensor_tensor(out=BR[:, blk, t0:t0 + T], in0=TF[q][:, :T], in1=pg[:, :T],
                                                              op=ALU.mult), r=[('TFs', q), pgk], w=brkeys(t0, T))
                S.barrier()
            if debug == 'sc' and l == 0:
                dump([(BR[:, g, :], LT) for g in range(2)])
                return 'stop'
            out_proj(l, 2, BR, WO, sl_wo, tiles_out, j)
            with ExitStack() as pd_:
                WMc = sb("WMc", [128, 8, 512], BF16, pd_)
                RAWc = [sb(f"RAWc{i}", [128, RAWW], BF16, pd_) for i in range(2)]
                DG = sb("DGc", [128, 31, 128], BF16, pd_)
                CV = sb("CV", [128, 2, LT], F32, pd_)
                TF = [sb(f"TFc{i}", [128, 512], F32, pd_) for i in range(2)]
                CVB = [sb(f"CVB{i}", [128, 512], BF16, pd_) for i in range(2)]
                SQB = [sb(f"SQB{i}", [128, 512], BF16, pd_) for i in range(2)]
                MEAN = sb("MEAN", [128, 512], F32, pd_)
                VAR = sb("VAR", [128, 512], F32, pd_)
                S.dma('pool', sl_wm, [(WMc[:], win[l, :, 2312:2824].rearrange("(k p) n -> p k n", p=128))], w=['WMc'])
                nq = 0
                for blk in range(2):
                    S.op('dve', lambda e: e.memset(RAWc[blk][:], 0.0), w=[('RAWc', blk)])
                    for (t0, T) in tiles_out:
                        q = nq % 2
                        nq += 1
                        pa_, pak = psum()
                        proj(pa_[:, :T], WMc, blk * 128, t0, T, 'WMc', pak)
                        pg, pgk = psum()
                        proj(pg[:, :T], WMc, 256 + blk * 128, t0, T, 'WMc', pgk)
                        S.op('act', lambda e: e.activation(out=TF[q][:, :T], in_=pg[:, :T], func=AF.Sigmoid), r=[pgk], w=[('TFc', q)])
                        S.op('dve', lambda e: e.tensor_tensor(out=RAWc[blk][:, rawcol(t0):rawcol(t0) + T], in0=TF[q][:, :T],
                                                              in1=pa_[:, :T], op=ALU.mult), r=[('TFc', q), pak], w=[('RAWc', blk)])
                    build_diag(DG, l, "fcw", blk, 31)
                    for (t0, T) in tiles_out:
                        pcv, pcvk = psum()
                        conv_mm(pcv[:, :T], DG, RAWc[blk], ('RAWc', blk), t0, T, 31, pcvk)
                        S.op('act', lambda e: e.activation(out=CV[:, blk, t0:t0 + T], in_=pcv[:, :T], func=AF.Identity,
                                                           bias=ppc("fcb", l, blk), scale=1.0), r=[pcvk, 'PP'], w=[('CV', blk, t0 // 512)])
                MEANs = [MEAN, sb("MEAN1", [128, 512], F32, pd_)]
                VARs = [VAR, sb("VAR1", [128, 512], F32, pd_)]
                CVBs = [CVB, [sb(f"CVBx{i}", [128, 512], BF16, pd_) for i in range(2)]]
                SQBs = [SQB, [sb(f"SQBx{i}", [128, 512], BF16, pd_) for i in range(2)]]
                TFs = [TF, [sb(f"TFx{i}", [128, 512], F32, pd_) for i in range(2)]]

                def cf_ln(t0, T, q):
                    MEAN_, VAR_, CVB_, SQB_, TF_ = MEANs[q], VARs[q], CVBs[q], SQBs[q], TFs[q]
                    tt = t0 // 512
                    p1, p1k = psum()
                    p2, p2k = psum()
                    for blk in range(2):
                        S.op('dve', lambda e: e.tensor_copy(out=CVB_[blk][:, :T], in_=CV[:, blk, t0:t0 + T]),
                             r=[('CV', blk, tt)], w=[('CVB', q, blk)])
                        S.op('act', lambda e: e.activation(out=SQB_[blk][:, :T], in_=CV[:, blk, t0:t0 + T], func=AF.Square),
                             r=[('CV', blk, tt)], w=[('SQB', q, blk)])
                    yield
                    for blk in range(2):
                        S.op('pe', lambda e: e.matmul(p1[:, :T], ONESB[:], CVB_[blk][:, :T], start=(blk == 0), stop=(blk == 1)),
                             r=[('CVB', q, blk), 'ONESB'], w=[p1k])
                    for blk in range(2):
                        S.op('pe', lambda e: e.matmul(p2[:, :T], ONESB[:], SQB_[blk][:, :T], start=(blk == 0), stop=(blk == 1)),
                             r=[('SQB', q, blk), 'ONESB'], w=[p2k])
                    yield
                    S.op('act', lambda e: e.activation(out=MEAN_[:, :T], in_=p1[:, :T], func=AF.Copy, scale=1.0 / 256), r=[p1k], w=[('MEAN', q)])
                    yield
                    S.op('dve', lambda e: e.tensor_tensor(out=VAR_[:, :T], in0=MEAN_[:, :T], in1=MEAN_[:, :T], op=ALU.mult),
                         r=[('MEAN', q)], w=[('VAR', q)])
                    yield
                    S.op('dve', lambda e: e.scalar_tensor_tensor(out=VAR_[:, :T], in0=p2[:, :T], scalar=1.0 / 256, in1=VAR_[:, :T],
                                                                 op0=ALU.mult, op1=ALU.subtract), r=[p2k, ('VAR', q)], w=[('VAR', q)])
                    yield
                    S.op('act', lambda e: e.activation(out=VAR_[:, :T], in_=VAR_[:, :T], func=AF.Sqrt, bias=EPS5[:, 0:1], scale=1.0),
                         r=[('VAR', q), 'EPS6'], w=[('VAR', q)])
                    yield
                    S.op('dve', lambda e: e.reciprocal(out=VAR_[:, :T], in_=VAR_[:, :T]), r=[('VAR', q)], w=[('VAR', q)])
                    yield
                    for blk in range(2):
                        S.op('dve', lambda e: e.tensor_tensor(out=TF_[blk][:, :T], in0=CV[:, blk, t0:t0 + T], in1=MEAN_[:, :T],
                                                              op=ALU.subtract), r=[('CV', blk, tt), ('MEAN', q)], w=[('TFc', q, blk)])
                    yield
                    for blk in range(2):
                        S.op('dve', lambda e: e.tensor_tensor(out=TF_[blk][:, :T], in0=TF_[blk][:, :T], in1=VAR_[:, :T], op=ALU.mult),
                             r=[('TFc', q, blk), ('VAR', q)], w=[('TFc', q, blk)])
                    yield
                    for blk in range(2):
                        S.op('act', lambda e: e.activation(out=BR[:, blk, t0:t0 + T], in_=TF_[blk][:, :T], func=AF.Silu,
                                                           bias=ppc("fnb", l, blk), scale=ppc("fng", l, blk)),
                             r=[('TFc', q, blk), 'PP'], w=brkeys(t0, T))

                for i0 in range(0, len(tiles_out), 2):
                    active = [cf_ln(t0, T, q) for q, (t0, T) in enumerate(tiles_out[i0:i0 + 2])]
                    while active:
                        for g_ in list(active):
                            try:
                                next(g_)
                            except StopIteration:
                                active.remove(g_)
                S.barrier()
            if debug == 'cf' and l == 0:
                dump([(BR[:, g, :], LT) for g in range(2)])
                return 'stop'
            out_proj(l, 3, BR, WO, sl_wo, tiles_out, j)
            S.barrier()
        return None

    def ffn_phase(l, j, tiles, moe):
        with ExitStack() as ph:
            NST = 2
            W1 = [sb(f"W1_{i}", [128, 8, 512], BF16, ph) for i in range(NST)]
            W3 = [sb(f"W3_{i}", [128, 8, 512], BF16, ph) for i in range(NST)]
            W2 = [sb(f"W2_{i}", [128, 4, 1024], BF16, ph) for i in range(NST)]
            sl_w = [S.slot(f"ffw{i}") for i in range(NST)]
            G = sb("G", [128, 4, L if moe else LT], BF16, ph)
            SA = [sb(f"SA{i}", [128, 512], F32, ph) for i in range(2)]
            CMB = sb("CMB", [128, L], BF16, ph) if moe else None
            nst = [0]
            nq = [0]

            def run(w1ap, w3ap, w2ap, F):
                f0 = 0
                while f0 < F:
                    fw = min(512, F - f0)
                    nb = fw // 128
                    s_ = nst[0] % NST
                    nst[0] += 1
                    S.dma('pool', sl_w[s_],
                          [(W1[s_][:, :, 0:fw], w1ap[:, f0:f0 + fw].rearrange("(k p) n -> p k n", p=128)),
                           (W3[s_][:, :, 0:fw], w3ap[:, f0:f0 + fw].rearrange("(k p) n -> p k n", p=128)),
                           (W2[s_][:, 0:nb, :], w2ap[f0:f0 + fw, :].rearrange("(k p) n -> p k n", p=128))],
                          w=[('FW', s_)])
                    for (t0, T) in tiles:
                        tt = t0 // 512
                        for jb in range(nb):
                            q = nq[0] % 2
                            nq[0] += 1
                            pa_, pak = psum()
                            for k in range(8):
                                S.op('pe', lambda e: e.matmul(pa_[:, :T], W1[s_][:, k, jb * 128:(jb + 1) * 128], H[:, k, t0:t0 + T],
                                                              start=(k == 0), stop=(k == 7)), r=[('FW', s_), ('H', k, tt)], w=[pak], inc=(k == 7))
                            pb2, pbk = psum()
                            for k in range(8):
                                S.op('pe', lambda e: e.matmul(pb2[:, :T], W3[s_][:, k, jb * 128:(jb + 1) * 128], H[:, k, t0:t0 + T],
                                                              start=(k == 0), stop=(k == 7)), r=[('FW', s_), ('H', k, tt)], w=[pbk], inc=(k == 7))
                            S.op('act', lambda e: e.activation(out=SA[q][:, :T], in_=pa_[:, :T], func=AF.Silu), r=[pak], w=[('SA', q)])
                            S.op('dve', lambda e: e.tensor_tensor(out=G[:, jb, t0:t0 + T], in0=SA[q][:, :T], in1=pb2[:, :T],
                                                                  op=ALU.mult), r=[('SA', q), pbk], w=[('G', jb, tt)])
                            if moe:
                                S.op('dve', lambda e: e.tensor_tensor(out=G[:, jb, t0:t0 + T], in0=G[:, jb, t0:t0 + T],
                                                                      in1=CMB[:, t0:t0 + T], op=ALU.mult),
                                     r=[('G', jb, tt), 'CMB'], w=[('G', jb, tt)])
                    for (t0, T) in tiles:
                        tt = t0 // 512
                        jj = j if t0 < L else 2
                        for kb in range(8):
                            po, pok = psum()
                            for jb in range(nb):
                                S.op('pe', lambda e: e.matmul(po[:, :T], W2[s_][:, jb, kb * 128:(kb + 1) * 128], G[:, jb, t0:t0 + T],
                                                              start=(jb == 0), stop=(jb == nb - 1)), r=[('FW', s_), ('G', jb, tt)], w=[pok], inc=(jb == nb - 1))
                            g_ap = MOD[:, l, 40 + kb, jj:jj + 1]
                            S.op('dve', lambda e: e.scalar_tensor_tensor(out=X[:, kb, t0:t0 + T], in0=po[:, :T], scalar=g_ap,
                                                                         in1=X[:, kb, t0:t0 + T], op0=ALU.mult, op1=ALU.add),
                                 r=[pok, ('X', kb, tt)], w=[('X', kb, tt)])
                    f0 += fw

            if not moe:
                run(dd("ffn_w1")[0], dd("ffn_w3")[0], dd("ffn_w2")[0], DFF)
            else:
                CMBT = moe
                for ex in range(NE):
                    for (t0, T) in tiles:
                        pc, pck = psum()
                        S.op('pe', lambda e: e.matmul(pc[:, :T], CST[0:8, C_S8 + ex * 128:C_S8 + (ex + 1) * 128], CMBT[:, t0:t0 + T],
                                                      start=True, stop=True), r=['CMBT', 'CST'], w=[pck])
                        S.op('act', lambda e: e.activation(out=CMB[:, t0:t0 + T], in_=pc[:, :T], func=AF.Copy), r=[pck], w=['CMB'])
                    run(dd("moe_w1")[0, ex], dd("moe_w3")[0, ex], dd("moe_w2")[0, ex], EFF)
            S.barrier()

    def route_phase(pl, plk, CMBT):
        with ExitStack() as ph:
            LG = sb("LG", [128, 16, 8], F32, ph)
            LG2 = sb("LG2", [128, 16, 8], F32, ph)
            EQ1 = sb("EQ1", [128, 16, 8], F32, ph)
            EQ2 = sb("EQ2", [128, 16, 8], F32, ph)
            M1 = sb("M1", [128, 16], F32, ph)
            M2 = sb("M2", [128, 16], F32, ph)
            P1 = sb("P1", [128, 16], F32, ph)
            P2 = sb("P2", [128, 16], F32, ph)
            CM = sb("CM", [128, 16, 8], F32, ph)
            bc = lambda t: t[:].unsqueeze(2).to_broadcast([128, 16, 8])
            S.op('dve', lambda e: e.tensor_copy(out=LG[:], in_=pl[:, 0:128].rearrange("p (s x) -> p s x", x=8)), r=[plk], w=['LG'])
            S.op('dve', lambda e: e.reduce_max(out=M1[:], in_=LG[:], axis=AX.X), r=['LG'], w=['M1'])
            S.op('dve', lambda e: e.tensor_tensor(out=EQ1[:], in0=LG[:], in1=bc(M1), op=ALU.is_equal), r=['LG', 'M1'], w=['EQ1'])
            S.op('dve', lambda e: e.scalar_tensor_tensor(out=LG2[:], in0=EQ1[:], scalar=-1e30, in1=LG[:], op0=ALU.mult, op1=ALU.add),
                 r=['EQ1', 'LG'], w=['LG2'])
            S.op('dve', lambda e: e.reduce_max(out=M2[:], in_=LG2[:], axis=AX.X), r=['LG2'], w=['M2'])
            S.op('dve', lambda e: e.tensor_tensor(out=EQ2[:], in0=LG2[:], in1=bc(M2), op=ALU.is_equal), r=['LG2', 'M2'], w=['EQ2'])
            S.op('dve', lambda e: e.tensor_tensor(out=M2[:], in0=M2[:], in1=M1[:], op=ALU.subtract), r=['M2', 'M1'], w=['M2'])
            S.op('act', lambda e: e.activation(out=P2[:], in_=M2[:], func=AF.Sigmoid), r=['M2'], w=['P2'])
            S.op('act', lambda e: e.activation(out=P1[:], in_=M2[:], func=AF.Sigmoid, scale=-1.0), r=['M2'], w=['P1'])
            S.op('dve', lambda e: e.tensor_tensor(out=EQ1[:], in0=EQ1[:], in1=bc(P1), op=ALU.mult), r=['EQ1', 'P1'], w=['EQ1'])
            S.op('dve', lambda e: e.tensor_tensor(out=EQ2[:], in0=EQ2[:], in1=bc(P2), op=ALU.mult), r=['EQ2', 'P2'], w=['EQ2'])
            S.op('dve', lambda e: e.tensor_tensor(out=CM[:], in0=EQ1[:], in1=EQ2[:], op=ALU.add), r=['EQ1', 'EQ2'], w=['CM'])
            for grp in range(4):
                pt, ptk = psum()
                for s4 in range(4):
                    sub = grp * 4 + s4
                    S.op('pe', lambda e: e.transpose(pt[0:8, s4 * 128:(s4 + 1) * 128], CM[:, sub, :], CST[:, C_ID:C_ID + 128]),
                         r=['CM', 'CST'], w=[ptk])
                S.op('act', lambda e: e.activation(out=CMBT[:, grp * 512:(grp + 1) * 512], in_=pt[0:8, :], func=AF.Copy),
                     r=[ptk], w=['CMBT'])
            S.barrier()


    def final_phase(b):
        with ExitStack() as ph:
            SQ = [sb(f"fSQ{i}", [128, 512], BF16, ph) for i in range(2)]
            RS = [sb(f"fRS{i}", [128, 512], F32, ph) for i in range(2)]
            OT = [sb(f"fOT{i}", [128, 8, 512], F32, ph) for i in range(2)]
            nq = 0
            for ti, (t0, T) in enumerate(TT_MAIN):
                tt = t0 // 512
                pss, pk = psum()
                for k in range(8):
                    q = nq % 2
                    nq += 1
                    S.op('act', lambda e: e.activation(out=SQ[q][:, :T], in_=X[:, k, t0:t0 + T], func=AF.Square),
                         r=[('X', k, tt)], w=[('fSQ', q)])
                    S.op('pe', lambda e: e.matmul(pss[:, :T], ONESB[:], SQ[q][:, :T], start=(k == 0), stop=(k == 7)),
                         r=[('fSQ', q), 'ONESB'], w=[pk])
                rq = ti % 2
                S.op('act', lambda e: e.activation(out=RS[rq][:, :T], in_=pss[:, :T], func=AF.Sqrt, bias=EPS6[:, 0:1],
                                                   scale=1.0 / D), r=[pk, 'EPS6'], w=[('fRS', rq)])
                S.op('dve', lambda e: e.reciprocal(out=RS[rq][:, :T], in_=RS[rq][:, :T]), r=[('fRS', rq)], w=[('fRS', rq)])
                for k in range(8):
                    g_ap = ppc("fin", 0, k)
                    S.op('dve', lambda e: e.scalar_tensor_tensor(out=OT[rq][:, k, :T], in0=X[:, k, t0:t0 + T], scalar=g_ap,
                                                                 in1=RS[rq][:, :T], op0=ALU.mult, op1=ALU.mult),
                         r=[('X', k, tt), ('fRS', rq), 'PP'], w=[('fOT', rq)])
                S.dma('sp', sl_out[rq], [(outT[b, :, t0:t0 + T].rearrange("(k p) t -> p k t", p=128), OT[rq][:, :, :T])],
                      r=[('fOT', rq)])
            if b + 1 < nseq:
                load_x(b + 1)
            S.barrier()

    import os
    if os.environ.get("OPLIMIT"):
        S.limit = int(os.environ["OPLIMIT"])
    for b in range(nseq):
      try:
          norm_phase(0, 0, b, TT_ALL)
          if debug == 'norm1':
              dump([(H[:, k, :], LT) for k in range(8)])
              break
          r_ = mixer_phase(0, b, False)
          if r_ == 'stop':
              break
          if debug == 'mix0':
              dump([(X[:, k, :], LT) for k in range(8)])
              break
          norm_phase(0, 1, b, TT_ALL)
          ffn_phase(0, b, TT_ALL, None)
          if debug == 'l0':
              dump([(X[:, k, :], LT) for k in range(8)])
              break
          norm_phase(1, 0, b, TT_ALL)
          r_ = mixer_phase(1, b, True)
          if debug == 'mix1':
              dump([(X[:, k, :], LT) for k in range(8)])
              break
          pl, plk = PS[5], ('ps', 5)
          st['ps'] = 0
          with ExitStack() as mo:
              CMBT = sb("CMBT", [8, L], F32, mo)
              norm_phase(1, 1, b, TT_MAIN, moe=(pl, plk))
              route_phase(pl, plk, CMBT)
              ffn_phase(1, b, TT_MAIN, CMBT)
          final_phase(b)
      except StopIteration:
        S.limit = None
        print('STOPPED at nops', S.nops)
        dump([(X[:, 0, :], LT)])
        break

    S.barrier()
    global USED
    USED = set(DECL.keys())
    return nc


def _host_layout(inp):
    f = lambda a: np.ascontiguousarray(np.asarray(a, dtype=np.float32))
    pp = np.zeros((128, NPP), np.float32)

    def blk(v):
        v = np.asarray(v, np.float32)
        return v.reshape(-1, 128).T

    for l in range(2):
        def put(name, arr):
            o = PPO[(name, l)]
            pp[:, o:o + arr.shape[1]] = arr
        put("n1g", blk(inp['norm1_g'][l]))
        put("n2g", blk(inp['norm2_g'][l]))
        cw = np.asarray(inp['ssd_conv_w'][l], np.float32)
        put("scw", np.stack([blk(cw[k]) for k in range(5)], axis=2).reshape(128, 30))
        put("scb", blk(inp['ssd_conv_b'][l]))
        put("sD", blk(np.repeat(np.asarray(inp['ssd_d'][l], np.float32), 64)))
        put("sng", blk(inp['ssd_norm_g'][l]))
        c3 = np.asarray(inp['sc_conv_w'][l], np.float32)
        put("ccw", np.stack([blk(c3[k]) for k in range(3)], axis=2).reshape(128, 6))
        c31 = np.asarray(inp['cf_conv_w'][l], np.float32)
        put("fcw", np.stack([blk(c31[k]) for k in range(31)], axis=2).reshape(128, 62))
        put("fcb", blk(inp['cf_conv_b'][l]))
        put("fng", blk(inp['cf_norm_g'][l]))
        put("fnb", blk(inp['cf_norm_b'][l]))
        put("gbs", np.asarray(inp['gm_bs'][l], np.float32).T)
        put("modb", blk(inp['mod_b'][l]))
    o = PPO[("fin", 0)]
    pp[:, o:o + 8] = blk(inp['final_norm_g'])
    rb = np.zeros((128, 2 * RBW), np.float32)
    for l in range(2):
        row = np.concatenate([np.asarray(inp['ssd_dt_bias'][l], np.float32).reshape(8),
                              np.asarray(inp['ssd_a_log'][l], np.float32).reshape(8),
                              np.asarray(inp['gm_norm_g'][l], np.float32), np.asarray(inp['gm_norm_b'][l], np.float32)])
        rb[:, l * RBW:(l + 1) * RBW] = row[None, :]
    cst = np.zeros((128, NCST), np.float32)
    i = np.arange(128)
    cst[:, C_ID:C_ID + 128] = np.eye(128)
    cst[:, C_TU:C_TU + 128] = (i[:, None] <= i[None, :])
    cst[:, C_TL:C_TL + 128] = (i[:, None] >= i[None, :])
    cst[:, C_MF:C_MF + 128] = np.where(i[None, :] >= i[:, None], 0.0, -1e30)
    cst[:, C_MB:C_MB + 128] = np.where(i[None, :] <= i[:, None], 0.0, -1e30)
    cst[:, C_ON:C_ON + 128] = 1.0
    for h in range(4):
        cst[h, C_S4 + h * 128:C_S4 + (h + 1) * 128] = 1.0
    for e in range(8):
        cst[e, C_S8 + e * 128:C_S8 + (e + 1) * 128] = 1.0
    rt = np.ascontiguousarray(np.asarray(inp['moe_router'][0], np.float32).reshape(8, 128, 8).transpose(1, 0, 2))
    ws = np.asarray(inp['gm_ws'], np.float32)
    wsT = np.ascontiguousarray(ws.transpose(3, 0, 1, 2)).reshape(128, 2 * 4 * 128)
    shared = {"pp": pp, "rb": rb, "cst": cst, "rt": rt, "wsT": wsT}
    for kname in ("mod_w", "w_in", "w_out", "ffn_w1", "ffn_w3", "ffn_w2", "moe_w1", "moe_w3", "moe_w2"):
        shared[kname] = f(inp[kname])
    return shared


def _core_inputs(inp, shared, core, nseq=2):
    x = np.asarray(inp['x'], np.float32)
    ctx = np.asarray(inp['ctx'], np.float32)
    c = np.asarray(inp['c'], np.float32)
    cc = np.asarray(inp['c_ctx'], np.float32)
    bs = [core * 2 + i for i in range(nseq)]
    m = dict(shared)
    m["xT"] = np.ascontiguousarray(np.stack([x[b].T for b in bs]))
    m["cxT"] = np.ascontiguousarray(np.stack([ctx[b].T for b in bs]))
    cols = [c[core * 2], c[core * 2 + 1], cc]
    m["cT"] = np.ascontiguousarray(np.stack([v.reshape(8, 128).T for v in cols], axis=2))
    return m


def kernel(**inp):
    shared = _host_layout(inp)
    nc = build(2)
    in_maps = [_core_inputs(inp, shared, c) for c in range(8)]
    res = run_bass_kernel_spmd(nc, in_maps, core_ids=list(range(8)))
    out = np.empty((16, L, D), np.float32)
    for c in range(8):
        o = res.results[c]["outT"]
        for i in range(2):
            out[c * 2 + i] = o[i].T
    return out
```

```python
import numpy as np
from contextlib import ExitStack
import concourse.bass as bass
import concourse.mybir as mybir
from concourse.bass_utils import run_bass_kernel_spmd

F32 = mybir.dt.float32
BF16 = mybir.dt.bfloat16
AF = mybir.ActivationFunctionType
ALU = mybir.AluOpType
AX = mybir.AxisListType

L = 2048
CT = 256
LT = L + CT
D = 1024
NCH = LT // 128
INC = 2824
DFF = 2816
EFF = 3584
NE = 8
TT_MAIN = [(0, 512), (512, 512), (1024, 512), (1536, 512)]
TT_ALL = TT_MAIN + [(2048, 256)]
PADC = 15
RAWW = PADC + L + PADC + CT + PADC


def rawcol(t0):
    return t0 + PADC if t0 < L else t0 + 2 * PADC


def _pp_layout():
    off = {}
    n = 0
    for l in range(2):
        for name, w in (("n1g", 8), ("n2g", 8), ("scw", 30), ("scb", 6), ("sD", 2), ("sng", 2),
                        ("ccw", 6), ("fcw", 62), ("fcb", 2), ("fng", 2), ("fnb", 2), ("gbs", 4), ("modb", 48)):
            off[(name, l)] = n
            n += w
    off[("fin", 0)] = n
    n += 8
    return off, n


PPO, NPP = _pp_layout()
RBW = 8 + 8 + 256 + 256
C_ID, C_TU, C_TL, C_MF, C_MB, C_ON, C_S4, C_S8 = 0, 128, 256, 384, 512, 640, 768, 1280
NCST = 1280 + 1024


import os as _os
FORCE_INC = bool(_os.environ.get('NOINC'))


class Sched:
    EPOCH = 28000

    def __init__(self, nc, self_sync=False):
        self.nc = nc
        self.eng = {'pe': nc.tensor, 'act': nc.scalar, 'dve': nc.vector, 'pool': nc.gpsimd, 'sp': nc.sync}
        self.semobj = {}
        self.cur = {}
        self.cnt = {}
        self.nid = 0
        self.self_sync = self_sync
        for e in self.eng:
            self._newsem(e)
        self.seen = {e: {} for e in self.eng}
        self.res = {}
        self.slots = []
        self.nwait = 0

    def _newsem(self, e):
        self.nid += 1
        key = (e, self.nid)
        self.semobj[key] = self.nc.alloc_semaphore(f"s_{e}_{self.nid}")
        self.cur[e] = key
        self.cnt[e] = 0

    def slot(self, name):
        self.nid += 1
        key = ('dma', self.nid)
        self.semobj[key] = self.nc.alloc_semaphore(f"d_{name}_{self.nid}")
        s = {'key': key, 'cnt': 0}
        self.slots.append(s)
        return s

    def _wait(self, e, toks):
        need = {}
        for t in toks:
            if t is None:
                continue
            k, v = t
            if k[0] == e and not (self.self_sync and e in ('act', 'dve')):
                continue
            if self.seen[e].get(k, 0) >= v:
                continue
            if need.get(k, 0) < v:
                need[k] = v
        for k, v in need.items():
            self.eng[e].wait_ge(self.semobj[k], v)
            self.seen[e][k] = v
            self.nwait += 1

    def _deps(self, r, w):
        toks = []
        for key in r:
            ent = self.res.get(key)
            if ent is not None and ent[0] is not None:
                toks.append(ent[0])
        for key in w:
            ent = self.res.get(key)
            if ent is not None:
                if ent[0] is not None:
                    toks.append(ent[0])
                toks.extend(ent[1].items())
        return toks

    def _record(self, tok, r, w):
        for k2 in w:
            self.res[k2] = [tok, {}]
        for k2 in r:
            ent = self.res.get(k2)
            if ent is None:
                ent = self.res[k2] = [None, {}]
            if ent[1].get(tok[0], 0) < tok[1]:
                ent[1][tok[0]] = tok[1]

    def op(self, e, fn, r=(), w=(), inc=True):
        self.nops = getattr(self, 'nops', 0) + 1
        if getattr(self, 'limit', None) is not None and self.nops > self.limit:
            raise StopIteration
        pr = [k for k in r if isinstance(k, tuple) and k[0] in ('ps', 'psb')]
        if pr:
            r = [k for k in r if k not in pr]
            w = list(w) + pr
        self._wait(e, self._deps(r, w))
        ins = fn(self.eng[e])
        key = self.cur[e]
        if not inc and not FORCE_INC:
            tok = (key, self.cnt[e] + 1)
            self._record(tok, r, w)
            return tok
        ins.then_inc(self.semobj[key], 1)
        self.cnt[e] += 1
        tok = (key, self.cnt[e])
        self._record(tok, r, w)
        if self.cnt[e] >= self.EPOCH:
            self._newsem(e)
        return tok

    def dma(self, q, slot, items, r=(), w=()):
        self._wait(q, self._deps(r, w))
        for (o, i) in items:
            self.eng[q].dma_start(out=o, in_=i).then_inc(self.semobj[slot['key']], 16)
            slot['cnt'] += 16
        tok = (slot['key'], slot['cnt'])
        self._record(tok, r, w)
        return tok

    def barrier(self):
        toks = [(self.cur[e], self.cnt[e]) for e in self.eng if self.cnt[e] > 0]
        toks += [(s['key'], s['cnt']) for s in self.slots if s['cnt'] > 0]
        for e in self.eng:
            self._wait(e, toks)
        self.res = {}


def build(nseq=2, debug=None, self_sync=True):
    nc = bass.Bass("TRN2", target_bir_lowering=False)
    S = Sched(nc, self_sync=self_sync)
    es = ExitStack()

    SHAPES = {"xT": [nseq, D, L], "cxT": [nseq, D, CT], "cT": [128, 8, 3], "pp": [128, NPP], "rb": [128, 2 * RBW],
              "cst": [128, NCST], "rt": [128, 8, 8], "wsT": [128, 2 * 4 * 128], "mod_w": [2, D, 6 * D],
              "w_in": [2, D, INC], "w_out": [2, D, D], "ffn_w1": [1, D, DFF], "ffn_w3": [1, D, DFF],
              "ffn_w2": [1, DFF, D], "moe_w1": [1, NE, D, EFF], "moe_w3": [1, NE, D, EFF], "moe_w2": [1, NE, EFF, D]}
    DECL = {}

    def dd(name):
        if name not in DECL:
            DECL[name] = nc.dram_tensor(name, SHAPES[name], F32, kind="ExternalInput").ap()
        return DECL[name]

    xT, cxT, cTd, ppd, rbd, cstd, rtd, wsTd, mod_w = (dd(n_) for n_ in
                                                      ("xT", "cxT", "cT", "pp", "rb", "cst", "rt", "wsT", "mod_w"))
    outT = nc.dram_tensor("outT", [nseq, D, L], F32, kind="ExternalOutput").ap()
    dbg = None
    if debug is not None:
        dbg = nc.dram_tensor("dbg", [128, 8 * LT], F32, kind="ExternalOutput").ap()

    uid = [0]

    def sb(name, shape, dt, stack=es):
        uid[0] += 1
        return stack.enter_context(nc.sbuf_tensor(f"{name}_{uid[0]}", shape, dt))

    X = sb("X", [128, 8, LT], F32)
    H = sb("H", [128, 8, LT], BF16)
    PP = sb("PP", [128, NPP], F32)
    RB = sb("RB", [128, 2 * RBW], F32)
    CST = sb("CST", [128, NCST], F32)
    RT = sb("RT", [128, 8, 8], F32)
    IDB = sb("IDB", [128, 128], BF16)
    ONESB = sb("ONESB", [128, 128], BF16)
    WST = sb("WSTb", [128, 2 * 4 * 128], BF16)
    CTs = sb("CTs", [128, 8, 3], F32)
    SCb = sb("SCb", [128, 8, 3], BF16)
    MOD = sb("MOD", [128, 2, 48, 3], F32)
    ATAB = sb("ATAB", [128, 2, 2, 8, 3], F32)
    NEGA = sb("NEGA", [128, 2, 8], F32)
    EPS6 = sb("EPS6", [128, 1], F32)
    EPS5 = sb("EPS5", [128, 1], F32)

    PS = [es.enter_context(nc.psum_tensor(f"ps{i}", [128, 512], F32)) for i in range(6)]
    PSB = [es.enter_context(nc.psum_tensor(f"psb{i}", [128, 1024], BF16)) for i in range(2)]
    st = {'ps': 0, 'psb': 0}

    def psum():
        i = st['ps']
        st['ps'] = (i + 1) % 6
        return PS[i], ('ps', i)

    def psumb():
        i = st['psb']
        st['psb'] = (i + 1) % 2
        return PSB[i], ('psb', i)

    sl_init = S.slot("init")
    sl_x = S.slot("x")
    sl_out = [S.slot("out0"), S.slot("out1")]
    sl_dbg = S.slot("dbg")
    sl_sel = S.slot("sel")

    def ppc(name, l, i=0, n=1):
        o = PPO[(name, l)] + i
        return PP[:, o:o + n]

    S.dma('sp', sl_init, [(PP[:], ppd), (RB[:], rbd), (CST[:], cstd), (RT[:], rtd), (CTs[:], cTd)],
          w=['PP', 'RB', 'CST', 'RT', 'CTs'])
    sl_init2 = S.slot("init2")
    S.dma('pool', sl_init2, [(IDB[:], cstd[:, C_ID:C_ID + 128]), (ONESB[:], cstd[:, C_ON:C_ON + 128]),
                             (WST[:], wsTd)], w=['IDB', 'ONESB', 'WST'])
    S.op('dve', lambda e: e.memset(EPS6[:], 1e-6), w=['EPS6'])
    S.op('dve', lambda e: e.memset(EPS5[:], 1e-5), w=['EPS6'])
    S.op('act', lambda e: e.activation(out=SCb[:], in_=CTs[:], func=AF.Silu), r=['CTs'], w=['SCb'])
    for l in range(2):
        S.op('act', lambda e: e.activation(out=NEGA[:, l, :], in_=RB[:, l * RBW + 8:l * RBW + 16], func=AF.Exp),
             r=['RB'], w=[('NEGA', l)])
        S.op('dve', lambda e: e.tensor_scalar(out=NEGA[:, l, :], in0=NEGA[:, l, :], scalar1=-1.0, scalar2=0.0,
                                              op0=ALU.mult, op1=ALU.add), r=[('NEGA', l)], w=[('NEGA', l)])

    def load_x(b):
        items = []
        for k in range(8):
            items.append((X[:, k, 0:L], xT[b, k * 128:(k + 1) * 128, :]))
            items.append((X[:, k, L:LT], cxT[b, k * 128:(k + 1) * 128, :]))
        S.dma('sp', sl_x, items, w=[('X', k, tt) for k in range(8) for tt in range(5)])

    load_x(0)
    with ExitStack() as ph:
        MW = [sb(f"MW{i}", [128, 8, 512], BF16, ph) for i in range(2)]
        sl_mw = [S.slot("mw0"), S.slot("mw1")]
        n = 0
        for l in range(2):
            pm, pmk = psum()
            for ct in range(12):
                s = n % 2
                n += 1
                S.dma('pool', sl_mw[s], [(MW[s][:], mod_w[l, :, ct * 512:(ct + 1) * 512]
                                          .rearrange("(k p) n -> p k n", p=128))], w=[('MW', s)])
                for blk in range(4):
                    col = ct * 4 + blk
                    for k in range(8):
                        S.op('pe', lambda e: e.matmul(pm[:, col * 3:col * 3 + 3], MW[s][:, k, blk * 128:(blk + 1) * 128],
                                                      SCb[:, k, :], start=(k == 0), stop=(k == 7)),
                             r=[('MW', s), 'SCb'], w=[pmk])
            mb = ppc("modb", l, 0, 48)
            S.op('dve', lambda e: e.tensor_tensor(out=MOD[:, l], in0=pm[:, 0:144].rearrange("p (c j) -> p c j", j=3),
                                                  in1=mb.unsqueeze(2).to_broadcast([128, 48, 3]), op=ALU.add),
                 r=[pmk, 'PP'], w=[('MOD', l)])
            for ni, (gname, sco) in enumerate((("n1g", 8), ("n2g", 32))):
                g = ppc(gname, l, 0, 8)
                S.op('dve', lambda e: e.scalar_tensor_tensor(out=ATAB[:, l, ni], in0=MOD[:, l, sco:sco + 8, :], scalar=1.0,
                                                             in1=g.unsqueeze(2).to_broadcast([128, 8, 3]),
                                                             op0=ALU.add, op1=ALU.mult),
                     r=[('MOD', l), 'PP'], w=[('ATAB', l, ni)])
        S.barrier()

    def dump(ap_list):
        o = 0
        items = []
        for ap, n_ in ap_list:
            items.append((dbg[:, o:o + n_], ap))
            o += n_
        S.barrier()
        tok = S.dma('pool', sl_dbg, items)
        S._wait('sp', [tok])

    def norm_phase(l, ni, j, tiles, moe=None):
        sh0 = 0 if ni == 0 else 24
        with ExitStack() as ph:
            SQ = [sb(f"SQ{i}", [128, 512], BF16, ph) for i in range(4)]
            RS = [sb(f"RS{i}", [128, 512], F32, ph) for i in range(2)]
            TMP = [sb(f"TMP{i}", [128, 512], F32, ph) for i in range(4)]
            H32s = [sb(f"H32_{i}", [128, 8, 512], F32, ph) for i in range(2)] if moe is not None else None

            def norm_tile(t0, T, g):
                tt = t0 // 512
                jj = j if t0 < L else 2
                H32 = H32s[g] if moe is not None else None
                pss, pk = psum()
                for k in range(8):
                    q = 2 * g + k % 2
                    S.op('act', lambda e: e.activation(out=SQ[q][:, :T], in_=X[:, k, t0:t0 + T], func=AF.Square),
                         r=[('X', k, tt)], w=[('SQ', q)])
                    S.op('pe', lambda e: e.matmul(pss[:, :T], ONESB[:], SQ[q][:, :T], start=(k == 0), stop=(k == 7)),
                         r=[('SQ', q), 'ONESB'], w=[pk])
                    if k % 2 == 1:
                        yield
                rq = g
                S.op('act', lambda e: e.activation(out=RS[rq][:, :T], in_=pss[:, :T], func=AF.Sqrt, bias=EPS6[:, 0:1],
                                                   scale=1.0 / D), r=[pk, 'EPS6'], w=[('RS', rq)])
                yield
                S.op('dve', lambda e: e.reciprocal(out=RS[rq][:, :T], in_=RS[rq][:, :T]), r=[('RS', rq)], w=[('RS', rq)])
                yield
                for k in range(8):
                    q = 2 * g + k % 2
                    S.op('dve', lambda e: e.tensor_tensor(out=TMP[q][:, :T], in0=X[:, k, t0:t0 + T], in1=RS[rq][:, :T],
                                                          op=ALU.mult), r=[('X', k, tt), ('RS', rq)], w=[('TMP', q)])
                    a_ap = ATAB[:, l, ni, k, jj:jj + 1]
                    b_ap = MOD[:, l, sh0 + k, jj:jj + 1]
                    if moe is None:
                        S.op('act', lambda e: e.activation(out=H[:, k, t0:t0 + T], in_=TMP[q][:, :T], func=AF.Identity,
                                                           bias=b_ap, scale=a_ap),
                             r=[('TMP', q)], w=[('H', k, tt)])
                    else:
                        S.op('act', lambda e: e.activation(out=H32[:, k, :T], in_=TMP[q][:, :T], func=AF.Identity,
                                                           bias=b_ap, scale=a_ap),
                             r=[('TMP', q)], w=[('H32', g, k)])
                        S.op('dve', lambda e: e.tensor_copy(out=H[:, k, t0:t0 + T], in_=H32[:, k, :T]),
                             r=[('H32', g, k)], w=[('H', k, tt)])
                    if k % 2 == 1:
                        yield
                if moe is not None:
                    pl, plk = moe
                    for sub in range(T // 128):
                        c0 = (t0 // 128 + sub) * 8
                        for k in range(8):
                            S.op('pe', lambda e: e.matmul(pl[:, c0:c0 + 8], H32[:, k, sub * 128:(sub + 1) * 128], RT[:, k, :],
                                                          start=(k == 0), stop=(k == 7)),
                                 r=[('H32', g, k), 'RT'], w=[plk])

            pending = [norm_tile(t0, T, i % 2) for i, (t0, T) in enumerate(tiles)]
            active = []
            while pending or active:
                if pending and len(active) < 2:
                    active.append(pending.pop(0))
                for g_ in list(active):
                    try:
                        next(g_)
                    except StopIteration:
                        active.remove(g_)
            S.barrier()


    def proj(ps_ap, W, c0, t0, T, wkey, pk, n=128):
        for k in range(8):
            S.op('pe', lambda e: e.matmul(ps_ap, W[:, k, c0:c0 + n], H[:, k, t0:t0 + T], start=(k == 0), stop=(k == 7)),
                 r=[wkey, ('H', k, t0 // 512)], w=[pk], inc=(k == 7))

    def brkeys(t0, T):
        return [('BR', c) for c in range(t0 // 128, (t0 + T) // 128)]

    def out_proj(l, br, BR, WO, sl_wo, tiles, j):
        S.dma('pool', sl_wo, [(WO[:], dd("w_out")[l, br * 256:(br + 1) * 256, :].rearrange("(k p) n -> p k n", p=128))],
              w=['WO'])
        for (t0, T) in tiles:
            tt = t0 // 512
            jj = j if t0 < L else 2
            for kb in range(8):
                po, pk = psum()
                for jb in range(2):
                    S.op('pe', lambda e: e.matmul(po[:, :T], WO[:, jb, kb * 128:(kb + 1) * 128], BR[:, jb, t0:t0 + T],
                                                  start=(jb == 0), stop=(jb == 1)), r=['WO'] + brkeys(t0, T), w=[pk], inc=(jb == 1))
                g_ap = MOD[:, l, 16 + kb, jj:jj + 1]
                S.op('dve', lambda e: e.scalar_tensor_tensor(out=X[:, kb, t0:t0 + T], in0=po[:, :T], scalar=g_ap,
                                                             in1=X[:, kb, t0:t0 + T], op0=ALU.mult, op1=ALU.add),
                     r=[pk, ('X', kb, tt)], w=[('X', kb, tt)])

    def out_proj_multi(l, parts, tiles, j):
        for (br, BRx, WOx, wokey, brname, slot) in parts:
            S.dma('pool', slot, [(WOx[:], dd("w_out")[l, br * 256:(br + 1) * 256, :].rearrange("(k p) n -> p k n", p=128))],
                  w=[wokey])
        n = 2 * len(parts)
        for (t0, T) in tiles:
            tt = t0 // 512
            jj = j if t0 < L else 2
            for kb in range(8):
                po, pk = psum()
                idx = 0
                for (br, BRx, WOx, wokey, brname, slot) in parts:
                    for jb in range(2):
                        S.op('pe', lambda e: e.matmul(po[:, :T], WOx[:, jb, kb * 128:(kb + 1) * 128], BRx[:, jb, t0:t0 + T],
                                                      start=(idx == 0), stop=(idx == n - 1)),
                             r=[wokey] + [(brname, c_) for c_ in range(t0 // 128, (t0 + T) // 128)], w=[pk], inc=(idx == n - 1))
                        idx += 1
                g_ap = MOD[:, l, 16 + kb, jj:jj + 1]
                S.op('dve', lambda e: e.scalar_tensor_tensor(out=X[:, kb, t0:t0 + T], in0=po[:, :T], scalar=g_ap,
                                                             in1=X[:, kb, t0:t0 + T], op0=ALU.mult, op1=ALU.add),
                     r=[pk, ('X', kb, tt)], w=[('X', kb, tt)])

    def build_diag(DG, l, name, blk, ntap):
        for k in range(ntap):
            wap = ppc(name, l, blk * ntap + k)
            S.op('pool', lambda e: e.tensor_scalar(out=DG[:, k, :], in0=IDB[:], scalar1=wap, scalar2=0.0,
                                                   op0=ALU.mult, op1=ALU.add), r=['IDB', 'PP'], w=['DG'])

    def conv_mm(ps_ap, DG, RAW, rkey, t0, T, ntap, pk):
        half = ntap // 2
        c0 = rawcol(t0)
        for k in range(ntap):
            S.op('pe', lambda e: e.matmul(ps_ap, DG[:, k, :], RAW[:, c0 + k - half:c0 + k - half + T],
                                          start=(k == 0), stop=(k == ntap - 1)), r=['DG', rkey], w=[pk], inc=(k == ntap - 1))

    def mixer_phase(l, j, last):
        tiles_out = TT_MAIN if last else TT_ALL
        win = dd("w_in")
        with ExitStack() as ph:
            WO = sb("WO", [128, 2, 1024], BF16, ph)
            BR = sb("BR", [128, 2, LT], BF16, ph)
            sl_wo = S.slot("wo")
            sl_wm = S.slot("wm")
            sl_wm2 = S.slot("wm2")

            with ExitStack() as pa:
                WMb = sb("WMb", [128, 8, 264], BF16, pa)
                XBC = sb("XBC", [128, 6, LT], BF16, pa)
                S.dma('pool', sl_wm2, [(WMb[:], win[l, :, 768:1032].rearrange("(k p) n -> p k n", p=128))], w=['WMb'])
                with ExitStack() as p1:
                    WMa = sb("WMa", [128, 8, 768], BF16, p1)
                    RAW = [sb(f"RAW{i}", [128, RAWW], BF16, p1) for i in range(2)]
                    DG = sb("DG", [128, 5, 128], BF16, p1)
                    S.dma('pool', sl_wm, [(WMa[:], win[l, :, 0:768].rearrange("(k p) n -> p k n", p=128))], w=['WMa'])
                    for i in range(2):
                        S.op('dve', lambda e: e.memset(RAW[i][:], 0.0), w=[('RAW', i)])
                    for blk in range(6):
                        rs = blk % 2
                        for (t0, T) in TT_ALL:
                            ps, pk = psum()
                            proj(ps[:, :T], WMa, blk * 128, t0, T, 'WMa', pk)
                            S.op('act', lambda e: e.activation(out=RAW[rs][:, rawcol(t0):rawcol(t0) + T], in_=ps[:, :T],
                                                               func=AF.Copy), r=[pk], w=[('RAW', rs)])
                        build_diag(DG, l, "scw", blk, 5)
                        for (t0, T) in TT_ALL:
                            ps, pk = psum()
                            conv_mm(ps[:, :T], DG, RAW[rs], ('RAW', rs), t0, T, 5, pk)
                            S.op('act', lambda e: e.activation(out=XBC[:, blk, t0:t0 + T], in_=ps[:, :T], func=AF.Silu,
                                                               bias=ppc("scb", l, blk), scale=1.0),
                                 r=[pk, 'PP'], w=[('XBC', blk, t0 // 512)])
                    S.barrier()
                if debug == 'xbc' and l == 0:
                    dump([(XBC[:, g, :], LT) for g in range(6)])
                    return 'stop'
                sm = {n_: sb("sm_" + n_, [128, NCH, 2, 4], F32, pa) for n_ in
                      ("DT", "DTA", "ACS", "TOTS", "DTE", "CD", "W2")}
                fl = lambda t: t[:].rearrange("p c d h -> p (c d h)")
                pd, pdk = psum()
                for c in range(NCH):
                    for k in range(8):
                        S.op('pe', lambda e: e.matmul(pd[:, c * 8:(c + 1) * 8], H[:, k, c * 128:(c + 1) * 128], WMb[:, k, 0:8],
                                                      start=(k == 0), stop=(k == 7)), r=['WMb', ('H', k, c // 4)], w=[pdk])
                dtb = RB[:, l * RBW:l * RBW + 8]
                S.op('dve', lambda e: e.tensor_tensor(out=sm["DT"][:].rearrange("p c d h -> p c (d h)"),
                                                      in0=pd[:, 0:NCH * 8].rearrange("p (c x) -> p c x", x=8),
                                                      in1=dtb.unsqueeze(1).to_broadcast([128, NCH, 8]), op=ALU.add),
                     r=[pdk, 'RB'], w=['DT'])
                S.op('act', lambda e: e.activation(out=fl(sm["DT"]), in_=fl(sm["DT"]), func=AF.Exp), r=['DT'], w=['DT'])
                S.op('act', lambda e: e.activation(out=fl(sm["DT"]), in_=fl(sm["DT"]), func=AF.Ln, bias=1.0, scale=1.0),
                     r=['DT'], w=['DT'])
                S.op('dve', lambda e: e.tensor_tensor(out=sm["DTA"][:].rearrange("p c d h -> p c (d h)"),
                                                      in0=sm["DT"][:].rearrange("p c d h -> p c (d h)"),
                                                      in1=NEGA[:, l, :].unsqueeze(1).to_broadcast([128, NCH, 8]), op=ALU.mult),
                     r=['DT', ('NEGA', l)], w=['DTA'])
                pf, pfk = psum()
                pb_, pbk = psum()
                pt, ptk = psum()
                n8 = NCH * 8
                S.op('pe', lambda e: e.matmul(pf[:, :n8], CST[:, C_TU:C_TU + 128], fl(sm["DTA"]), start=True, stop=True),
                     r=['DTA', 'CST'], w=[pfk])
                S.op('pe', lambda e: e.matmul(pb_[:, :n8], CST[:, C_TL:C_TL + 128], fl(sm["DTA"]), start=True, stop=True),
                     r=['DTA', 'CST'], w=[pbk])
                S.op('pe', lambda e: e.matmul(pt[:, :n8], CST[:, C_ON:C_ON + 128], fl(sm["DTA"]), start=True, stop=True),
                     r=['DTA', 'CST'], w=[ptk])
                v4 = lambda p_: p_[:, :n8].rearrange("p (c d h) -> p c d h", d=2, h=4)
                S.op('dve', lambda e: e.tensor_copy(out=sm["ACS"][:, :, 0, :], in_=v4(pf)[:, :, 0, :]), r=[pfk], w=['ACS'])
                S.op('dve', lambda e: e.tensor_copy(out=sm["ACS"][:, :, 1, :], in_=v4(pb_)[:, :, 1, :]), r=[pbk], w=['ACS'])
                S.op('dve', lambda e: e.tensor_copy(out=fl(sm["TOTS"]), in_=pt[:, :n8]), r=[ptk], w=['TOTS'])
                S.op('dve', lambda e: e.tensor_tensor(out=fl(sm["DTE"]), in0=fl(sm["TOTS"]), in1=fl(sm["ACS"]),
                                                      op=ALU.subtract), r=['TOTS', 'ACS'], w=['DTE'])
                S.op('act', lambda e: e.activation(out=fl(sm["DTE"]), in_=fl(sm["DTE"]), func=AF.Exp), r=['DTE'], w=['DTE'])
                S.op('act', lambda e: e.activation(out=fl(sm["CD"]), in_=fl(sm["TOTS"]), func=AF.Exp), r=['TOTS'], w=['CD'])
                S.op('dve', lambda e: e.tensor_tensor(out=fl(sm["W2"]), in0=fl(sm["DT"]), in1=fl(sm["DTE"]), op=ALU.mult),
                     r=['DT', 'DTE'], w=['W2'])
                if debug == 'dt' and l == 0:
                    dump([(fl(sm[n_]), NCH * 8) for n_ in ("DT", "DTA", "ACS", "TOTS", "DTE", "CD", "W2")])
                    return 'stop'
                XDTM = [[sb(f"XDTM{d}{hh}", [128, 2, 128], BF16, pa) for hh in range(2)] for d in range(2)]
                ENTM = [[sb(f"ENTM{d}{hh}", [128, 2, 128], BF16, pa) for hh in range(2)] for d in range(2)]
                STATE = [sb(f"STATE{d}", [128, 256], F32, pa) for d in range(2)]
                XDTD = [sb(f"XDTD{d}", [128, 256], BF16, pa) for d in range(2)]
                BTOK = [sb(f"BTOK{d}", [128, 256], BF16, pa) for d in range(2)]
                ACSTc = [sb(f"ACSTc{d}", [4, 128], F32, pa) for d in range(2)]
                DM = [sb(f"DM{d}", [128, 4, 128], F32, pa) for d in range(2)]
                LM = [sb(f"LM{d}", [128, 4, 128], BF16, pa) for d in range(2)]
                EB = [sb(f"EB{d}", [128, 4, 128], BF16, pa) for d in range(2)]
                MT = [sb(f"MT{d}", [128, 4, 128], BF16, pa) for d in range(2)]
                CE = [sb(f"CE{d}", [128, 4, 128], BF16, pa) for d in range(2)]
                YT_ = [sb(f"YT{d}", [128, 2, 128], F32, pa) for d in range(2)]
                ZS_ = [sb(f"ZS{d}", [128, 2, 128], F32, pa) for d in range(2)]
                SQy_ = [sb(f"SQy{d}", [128, 2, 128], BF16, pa) for d in range(2)]
                RSy_ = [sb(f"RSy{d}", [128, 128], F32, pa) for d in range(2)]
                for d in range(2):
                    for hh in range(2):
                        S.op('dve', lambda e: e.memset(XDTM[d][hh][:], 0.0), w=[('XDTM', d, hh)])
                        S.op('dve', lambda e: e.memset(ENTM[d][hh][:], 0.0), w=[('ENTM', d, hh)])
                    S.op('dve', lambda e: e.memset(STATE[d][:], 0.0), w=[('STATE', d)])
                orders = [[16, 17] + list(range(16)), [17, 16] + list(range(15, -1, -1))]
                pos = [{c: i for i, c in enumerate(orders[d])} for d in range(2)]

                def chunk_step(d, c):
                    tri = CST[:, C_TU:C_TU + 128] if d == 0 else CST[:, C_TL:C_TL + 128]
                    mneg = CST[:, C_MF:C_MF + 128] if d == 0 else CST[:, C_MB:C_MB + 128]
                    bA, bAk = PS[3 * d], ('ps', 3 * d)
                    bB, bBk = PS[3 * d + 1], ('ps', 3 * d + 1)
                    bC, bCk = PS[3 * d + 2], ('ps', 3 * d + 2)
                    pT, pTk = PSB[d], ('psb', d)
                    cs = slice(c * 128, (c + 1) * 128)
                    tt = c // 4
                    want_y = (c < 16) or (not last)
                    final = pos[d][c] > pos[1 - d][c]
                    YT, ZS, SQy, RSy = YT_[d], ZS_[d], SQy_[d], RSy_[d]
                    kYT, kZS, kSQ, kRS = ('YT', d), ('ZS', d), ('SQy', d), ('RSy', d)
                    for i4 in range(4):
                        S.op('pe', lambda e: e.transpose(pT[:, i4 * 128:(i4 + 1) * 128], XBC[:, i4, cs], IDB[:]),
                             r=[('XBC', i4, tt), 'IDB'], w=[pTk])
                    if want_y:
                        S.op('pe', lambda e: e.matmul(bB[0:4, 0:128], sm["DTA"][:, c, d, :], tri, start=True, stop=True),
                             r=['DTA', 'CST'], w=[bBk])
                    yield
                    w2 = sm["W2"][:, c, d, :]
                    S.op('dve', lambda e: e.tensor_tensor(out=XDTD[d][:].rearrange("p (h x) -> p h x", x=64),
                                                          in0=pT[:, 0:256].rearrange("p (h x) -> p h x", x=64),
                                                          in1=w2.unsqueeze(2).to_broadcast([128, 4, 64]), op=ALU.mult),
                         r=[pTk, 'W2'], w=[('XDTD', d)])
                    S.op('act', lambda e: e.activation(out=BTOK[d][:], in_=pT[:, 256:512], func=AF.Copy), r=[pTk], w=[('BTOK', d)])
                    if want_y:
                        S.op('act', lambda e: e.activation(out=ACSTc[d][:], in_=bB[0:4, 0:128], func=AF.Copy),
                             r=[bBk], w=[('ACSTc', d)])
                        w1 = sm["DT"][:, c, d, :].rearrange("p (g hh) -> p g hh", hh=2)
                        for hh in range(2):
                            S.op('dve', lambda e: e.tensor_tensor(
                                out=XDTM[d][hh][:, :, hh * 64:(hh + 1) * 64],
                                in0=pT[:, 0:256].rearrange("p (g hh x) -> p g hh x", g=2, hh=2)[:, :, hh, :],
                                in1=w1[:, :, hh].unsqueeze(2).to_broadcast([128, 2, 64]), op=ALU.mult),
                                r=[pTk, 'DT'], w=[('XDTM', d, hh)])
                    yield
                    for g in range(2):
                        S.op('pe', lambda e: e.matmul(bA[:, g * 128:(g + 1) * 128], BTOK[d][:, g * 128:(g + 1) * 128],
                                                      XDTD[d][:, g * 128:(g + 1) * 128], start=True, stop=True),
                             r=[('BTOK', d), ('XDTD', d)], w=[bAk])
                    if want_y:
                        for g in range(2):
                            S.op('pe', lambda e: e.matmul(bA[:, 256 + g * 128:256 + (g + 1) * 128], XBC[:, 2 + g, cs],
                                                          XBC[:, 4 + g, cs], start=True, stop=True),
                                 r=[('XBC', 2 + g, tt), ('XBC', 4 + g, tt)], w=[bAk])
                        for h in range(4):
                            S.op('pe', lambda e: e.matmul(bB[:, h * 128:(h + 1) * 128],
                                                          CST[0:4, C_S4 + h * 128:C_S4 + (h + 1) * 128], ACSTc[d][:],
                                                          start=True, stop=True), r=[('ACSTc', d), 'CST'], w=[bBk])
                        yield
                        pA3 = bB[:].rearrange("p (h x) -> p h x", x=128)
                        acs_c = sm["ACS"][:, c, d, :]
                        S.op('dve', lambda e: e.tensor_tensor(out=DM[d][:], in0=pA3,
                                                              in1=acs_c.unsqueeze(2).to_broadcast([128, 4, 128]),
                                                              op=ALU.subtract), r=[bBk, 'ACS'], w=[('DM', d)])
                        S.op('act', lambda e: e.activation(out=EB[d][:], in_=pA3, func=AF.Exp), r=[bBk], w=[('EB', d)])
                        S.op('dve', lambda e: e.tensor_tensor(out=DM[d][:], in0=DM[d][:],
                                                              in1=mneg.unsqueeze(1).to_broadcast([128, 4, 128]),
                                                              op=ALU.add), r=[('DM', d), 'CST'], w=[('DM', d)])
                        yield
                        S.op('act', lambda e: e.activation(out=LM[d][:], in_=DM[d][:], func=AF.Exp), r=[('DM', d)], w=[('LM', d)])
                        for g in range(2):
                            S.op('dve', lambda e: e.tensor_tensor(
                                out=CE[d][:, 2 * g:2 * g + 2, :], in0=EB[d][:, 2 * g:2 * g + 2, :],
                                in1=XBC[:, 4 + g, cs].unsqueeze(1).to_broadcast([128, 2, 128]),
                                op=ALU.mult), r=[('EB', d), ('XBC', 4 + g, tt)], w=[('CE', d)])
                        yield
                        for g in range(2):
                            S.op('dve', lambda e: e.tensor_tensor(
                                out=MT[d][:, 2 * g:2 * g + 2, :], in0=LM[d][:, 2 * g:2 * g + 2, :],
                                in1=bA[:, 256 + g * 128:256 + (g + 1) * 128].unsqueeze(1).to_broadcast([128, 2, 128]),
                                op=ALU.mult), r=[('LM', d), bAk], w=[('MT', d)])
                        yield
                        for g in range(2):
                            ops = [(XDTM[d][0][:, g, :], MT[d][:, 2 * g, :]), (XDTM[d][1][:, g, :], MT[d][:, 2 * g + 1, :]),
                                   (ENTM[d][0][:, g, :], CE[d][:, 2 * g, :]), (ENTM[d][1][:, g, :], CE[d][:, 2 * g + 1, :])]
                            for oi, (lt_, rh_) in enumerate(ops):
                                S.op('pe', lambda e: e.matmul(bC[:, g * 128:(g + 1) * 128], lt_, rh_,
                                                              start=(oi == 0), stop=(oi == 3)),
                                     r=[('XDTM', d, 0), ('XDTM', d, 1), ('ENTM', d, 0), ('ENTM', d, 1), ('MT', d), ('CE', d)],
                                     w=[bCk])
                        if final:
                            for g in range(2):
                                proj(bC[:, 256 + g * 128:256 + (g + 1) * 128], WMb, 8 + g * 128, c * 128, 128, 'WMb', bCk)
                        yield
                    cd = sm["CD"][:, c, d, :]
                    st3 = STATE[d][:].rearrange("p (h x) -> p h x", x=64)
                    S.op('dve', lambda e: e.tensor_tensor(out=st3, in0=st3, in1=cd.unsqueeze(2).to_broadcast([128, 4, 64]),
                                                          op=ALU.mult), r=[('STATE', d), 'CD'], w=[('STATE', d)])
                    S.op('dve', lambda e: e.tensor_tensor(out=STATE[d][:], in0=STATE[d][:], in1=bA[:, 0:256], op=ALU.add),
                         r=[('STATE', d), bAk], w=[('STATE', d)])
                    yield
                    for hh in range(2):
                        S.op('act', lambda e: e.activation(
                            out=ENTM[d][hh][:, :, hh * 64:(hh + 1) * 64],
                            in_=STATE[d][:].rearrange("p (g hh x) -> p g hh x", g=2, hh=2)[:, :, hh, :], func=AF.Copy),
                            r=[('STATE', d)], w=[('ENTM', d, hh)])
                    if want_y:
                        pY3 = bC[:, 0:256].rearrange("p (g x) -> p g x", x=128)
                        if not final:
                            S.op('act', lambda e: e.activation(out=BR[:, :, cs], in_=pY3, func=AF.Copy),
                                 r=[bCk], w=[('BR', c)])
                        else:
                            S.op('dve', lambda e: e.tensor_tensor(out=YT[:], in0=pY3, in1=BR[:, :, cs], op=ALU.add),
                                 r=[bCk, ('BR', c)], w=[kYT])
                            S.op('act', lambda e: e.activation(out=ZS[:], in_=bC[:, 256:512].rearrange("p (g x) -> p g x", x=128),
                                                               func=AF.Silu), r=[bCk], w=[kZS])
                            yield
                            for g in range(2):
                                S.op('dve', lambda e: e.scalar_tensor_tensor(
                                    out=YT[:, g, :], in0=XBC[:, g, cs], scalar=ppc("sD", l, g), in1=YT[:, g, :],
                                    op0=ALU.mult, op1=ALU.add), r=[('XBC', g, tt), kYT, 'PP'], w=[kYT])
                            S.op('dve', lambda e: e.tensor_tensor(out=YT[:], in0=YT[:], in1=ZS[:], op=ALU.mult),
                                 r=[kYT, kZS], w=[kYT])
                            yield
                            S.op('act', lambda e: e.activation(out=SQy[:], in_=YT[:], func=AF.Square), r=[kYT], w=[kSQ])
                            yield
                            for g in range(2):
                                S.op('pe', lambda e: e.matmul(bB[:, 0:128], ONESB[:], SQy[:, g, :], start=(g == 0), stop=(g == 1)),
                                     r=[kSQ, 'ONESB'], w=[bBk])
                            yield
                            S.op('act', lambda e: e.activation(out=RSy[:], in_=bB[:, 0:128], func=AF.Sqrt, bias=EPS6[:, 0:1],
                                                               scale=1.0 / 256), r=[bBk, 'EPS6'], w=[kRS])
                            yield
                            S.op('dve', lambda e: e.reciprocal(out=RSy[:], in_=RSy[:]), r=[kRS], w=[kRS])
                            for g in range(2):
                                S.op('dve', lambda e: e.scalar_tensor_tensor(
                                    out=BR[:, g, cs], in0=YT[:, g, :], scalar=ppc("sng", l, g), in1=RSy[:],
                                    op0=ALU.mult, op1=ALU.mult), r=[kYT, kRS, 'PP'], w=[('BR', c)])

                for i in range(NCH):
                    active = [chunk_step(0, orders[0][i]), chunk_step(1, orders[1][i])]
                    import os
                    if os.environ.get("NOINTER"):
                        for g_ in active:
                            for _ in g_:
                                pass
                        active = []
                    while active:
                        for g_ in list(active):
                            try:
                                next(g_)
                            except StopIteration:
                                active.remove(g_)
                S.barrier()
            if debug == 'ssd' and l == 0:
                dump([(BR[:, g, :], LT) for g in range(2)])
                return 'stop'
            out_chunks = list(range(16)) + ([] if last else [16, 17])
            with ExitStack() as pb:
                WMg = sb("WMg", [128, 8, 512], BF16, pb)
                U = sb("U", [128, 2, LT], F32, pb)
                VG = sb("VG", [128, 256], F32, pb)
                VG2 = sb("VG2", [128, 256], F32, pb)
                BST = sb("BST", [128, 6], F32, pb)
                MV = sb("MV", [128, 2], F32, pb)
                RSg = sb("RSg", [128, 1], F32, pb)
                VB = sb("VB", [128, 256], BF16, pb)
                SBb = sb("SBb", [128, 256], BF16, pb)
                BR2 = sb("BR2", [128, 2, LT], BF16, pb)
                WO2 = sb("WO2", [128, 2, 1024], BF16, pb)
                sl_wo2 = S.slot("wo2")
                S.dma('pool', sl_wm, [(WMg[:], win[l, :, 1032:1544].rearrange("(k p) n -> p k n", p=128))], w=['WMg'])
                for blk in range(2):
                    for (t0, T) in tiles_out:
                        ps, pk = psum()
                        proj(ps[:, :T], WMg, blk * 128, t0, T, 'WMg', pk)
                        S.op('act', lambda e: e.activation(out=U[:, blk, t0:t0 + T], in_=ps[:, :T], func=AF.Gelu_apprx_tanh),
                             r=[pk], w=[('U', t0 // 512)])
                gng = RB[:, l * RBW + 16:l * RBW + 272]
                gnb = RB[:, l * RBW + 272:l * RBW + 528]
                NG = 6
                VGs = [VG, VG2] + [sb(f"VGx{i}", [128, 256], F32, pb) for i in range(2 * NG - 2)]
                VBs = [VB] + [sb(f"VBx{i}", [128, 256], BF16, pb) for i in range(NG - 1)]
                SBs = [SBb] + [sb(f"SBx{i}", [128, 256], BF16, pb) for i in range(NG - 1)]
                MVs = [MV] + [sb(f"MVx{i}", [128, 2], F32, pb) for i in range(NG - 1)]
                RSs = [RSg] + [sb(f"RSx{i}", [128, 1], F32, pb) for i in range(NG - 1)]

                def gm_chunk(c, q):
                    VG_, VG2_, VB_, SB_, MV_, RS_ = VGs[2 * q], VGs[2 * q + 1], VBs[q], SBs[q], MVs[q], RSs[q]
                    kv, kv2, kvb, ksb, kmv, kmv2, krs = [(n_, q) for n_ in ('VG', 'VG2', 'VB', 'SBb', 'MV', 'MV2', 'RSg')]
                    cs = slice(c * 128, (c + 1) * 128)
                    pv, pvk = psum()
                    for k in range(8):
                        S.op('pe', lambda e: e.matmul(pv[:, 0:256], H[:, k, cs], WMg[:, k, 256:512], start=(k == 0), stop=(k == 7)),
                             r=['WMg', ('H', k, c // 4)], w=[pvk])
                    yield
                    S.op('act', lambda e: e.activation(out=VG_[:], in_=pv[:, 0:256], func=AF.Gelu_apprx_tanh), r=[pvk], w=[kv])
                    yield
                    S.op('dve', lambda e: e.reduce_sum(out=MV_[:, 0:1], in_=VG_[:], axis=AX.X), r=[kv], w=[kmv])
                    yield
                    S.op('dve', lambda e: e.tensor_scalar(out=MV_[:, 0:1], in0=MV_[:, 0:1], scalar1=-1.0 / 256, scalar2=0.0,
                                                          op0=ALU.mult, op1=ALU.add), r=[kmv], w=[kmv])
                    yield
                    S.op('dve', lambda e: e.tensor_scalar(out=VG_[:], in0=VG_[:], scalar1=MV_[:, 0:1], scalar2=0.0,
                                                          op0=ALU.add, op1=ALU.add), r=[kv, kmv], w=[kv])
                    yield
                    S.op('dve', lambda e: e.tensor_tensor(out=VG2_[:], in0=VG_[:], in1=VG_[:], op=ALU.mult), r=[kv], w=[kv2])
                    yield
                    S.op('dve', lambda e: e.reduce_sum(out=MV_[:, 1:2], in_=VG2_[:], axis=AX.X), r=[kv2], w=[kmv2])
                    yield
                    S.op('act', lambda e: e.activation(out=RS_[:], in_=MV_[:, 1:2], func=AF.Sqrt, bias=EPS5[:, 0:1], scale=1.0 / 256),
                         r=[kmv2, 'EPS6'], w=[krs])
                    yield
                    S.op('dve', lambda e: e.reciprocal(out=RS_[:], in_=RS_[:]), r=[krs], w=[krs])
                    yield
                    S.op('dve', lambda e: e.tensor_scalar(out=VG_[:], in0=VG_[:], scalar1=RS_[:, 0:1], scalar2=0.0,
                                                          op0=ALU.mult, op1=ALU.add), r=[kv, krs], w=[kv])
                    yield
                    S.op('dve', lambda e: e.tensor_tensor(out=VG_[:], in0=VG_[:], in1=gng, op=ALU.mult), r=[kv, 'RB'], w=[kv])
                    yield
                    S.op('dve', lambda e: e.tensor_tensor(out=VB_[:], in0=VG_[:], in1=gnb, op=ALU.add), r=[kv, 'RB'], w=[kvb])
                    yield
                    pss_, psk = psum()
                    for g in range(4):
                        S.op('pe', lambda e: e.matmul(pss_[:, g * 64:(g + 1) * 64], WST[:, (l * 4 + g) * 128:(l * 4 + g + 1) * 128],
                                                      VB_[:, g * 64:(g + 1) * 64], start=True, stop=True), r=[kvb, 'WST'], w=[psk])
                    yield
                    S.op('dve', lambda e: e.tensor_tensor(out=SB_[:].rearrange("p (g x) -> p g x", x=64),
                                                          in0=pss_[:, 0:256].rearrange("p (g x) -> p g x", x=64),
                                                          in1=ppc("gbs", l, 0, 4).unsqueeze(2).to_broadcast([128, 4, 64]),
                                                          op=ALU.add), r=[psk, 'PP'], w=[ksb])
                    yield
                    pT, pTk = psum()
                    pTb = pT[:].bitcast(BF16)
                    for blk in range(2):
                        S.op('pe', lambda e: e.transpose(pTb[:, blk * 128:(blk + 1) * 128], SB_[:, blk * 128:(blk + 1) * 128], IDB[:]),
                             r=[ksb, 'IDB'], w=[pTk])
                    yield
                    S.op('dve', lambda e: e.tensor_tensor(out=BR2[:, :, cs], in0=U[:, :, cs],
                                                          in1=pTb[:, 0:256].rearrange("p (g x) -> p g x", x=128), op=ALU.mult),
                         r=[('U', c // 4), pTk], w=[('BR2', c)])

                pend = list(out_chunks)
                free_q = list(range(NG))
                active = []
                while pend or active:
                    while pend and free_q:
                        q_ = free_q.pop(0)
                        active.append((gm_chunk(pend.pop(0), q_), q_))
                    for it in list(active):
                        try:
                            next(it[0])
                        except StopIteration:
                            active.remove(it)
                            free_q.append(it[1])
                if debug == 'gm' and l == 0:
                    dump([(BR2[:, g, :], LT) for g in range(2)])
                    return 'stop'
                out_proj_multi(l, [(0, BR, WO, 'WO', 'BR', sl_wo), (1, BR2, WO2, 'WO2', 'BR2', sl_wo2)], tiles_out, j)
                S.barrier()
            with ExitStack() as pc_:
                WMs = sb("WMs", [128, 8, 768], BF16, pc_)
                RAW = sb("RAWs", [128, RAWW], BF16, pc_)
                DG = sb("DGs", [128, 3, 128], BF16, pc_)
                TF = [sb(f"TFs{i}", [128, 512], F32, pc_) for i in range(2)]
                S.dma('pool', sl_wm, [(WMs[:], win[l, :, 1544:2312].rearrange("(k p) n -> p k n", p=128))], w=['WMs'])
                S.op('dve', lambda e: e.memset(RAW[:], 0.0), w=['RAWs'])
                nq = 0
                for blk in range(2):
                    for (t0, T) in tiles_out:
                        q = nq % 2
                        nq += 1
                        pc, pck = psum()
                        proj(pc[:, :T], WMs, 256 + blk * 128, t0, T, 'WMs', pck)
                        px, pxk = psum()
                        proj(px[:, :T], WMs, 512 + blk * 128, t0, T, 'WMs', pxk)
                        S.op('act', lambda e: e.activation(out=TF[q][:, :T], in_=pc[:, :T], func=AF.Copy), r=[pck], w=[('TFs', q)])
                        S.op('dve', lambda e: e.tensor_tensor(out=RAW[:, rawcol(t0):rawcol(t0) + T], in0=TF[q][:, :T],
                                                              in1=px[:, :T], op=ALU.mult), r=[('TFs', q), pxk], w=['RAWs'])
                    build_diag(DG, l, "ccw", blk, 3)
                    for (t0, T) in tiles_out:
                        q = nq % 2
                        nq += 1
                        pcv, pcvk = psum()
                        conv_mm(pcv[:, :T], DG, RAW, 'RAWs', t0, T, 3, pcvk)
                        pg, pgk = psum()
                        proj(pg[:, :T], WMs, blk * 128, t0, T, 'WMs', pgk)
                        S.op('act', lambda e: e.activation(out=TF[q][:, :T], in_=pcv[:, :T], func=AF.Copy), r=[pcvk], w=[('TFs', q)])
                        S.op('dve', lambda e: e.tensor_tensor(out=BR[:, blk, t0:t0 + T], in0=TF[q][:, :T], in1=pg[:, :T],
                                                              op=ALU.mult), r=[('TFs', q), pgk], w=brkeys(t0, T))
                S.barrier()
            if debug == 'sc' and l == 0:
                dump([(BR[:, g, :], LT) for g in range(2)])
                return 'stop'
            out_proj(l, 2, BR, WO, sl_wo, tiles_out, j)
            with ExitStack() as pd_:
                WMc = sb("WMc", [128, 8, 512], BF16, pd_)
                RAWc = [sb(f"RAWc{i}", [128, RAWW], BF16, pd_) for i in range(2)]
                DG = sb("DGc", [128, 31, 128], BF16, pd_)
                CV = sb("CV", [128, 2, LT], F32, pd_)
                TF = [sb(f"TFc{i}", [128, 512], F32, pd_) for i in range(2)]
                CVB = [sb(f"CVB{i}", [128, 512], BF16, pd_) for i in range(2)]
                SQB = [sb(f"SQB{i}", [128, 512], BF16, pd_) for i in range(2)]
                MEAN = sb("MEAN", [128, 512], F32, pd_)
                VAR = sb("VAR", [128, 512], F32, pd_)
                S.dma('pool', sl_wm, [(WMc[:], win[l, :, 2312:2824].rearrange("(k p) n -> p k n", p=128))], w=['WMc'])
                nq = 0
                for blk in range(2):
                    S.op('dve', lambda e: e.memset(RAWc[blk][:], 0.0), w=[('RAWc', blk)])
                    for (t0, T) in tiles_out:
                        q = nq % 2
                        nq += 1
                        pa_, pak = psum()
                        proj(pa_[:, :T], WMc, blk * 128, t0, T, 'WMc', pak)
                        pg, pgk = psum()
                        proj(pg[:, :T], WMc, 256 + blk * 128, t0, T, 'WMc', pgk)
                        S.op('act', lambda e: e.activation(out=TF[q][:, :T], in_=pg[:, :T], func=AF.Sigmoid), r=[pgk], w=[('TFc', q)])
                        S.op('dve', lambda e: e.tensor_tensor(out=RAWc[blk][:, rawcol(t0):rawcol(t0) + T], in0=TF[q][:, :T],
                                                              in1=pa_[:, :T], op=ALU.mult), r=[('TFc', q), pak], w=[('RAWc', blk)])
                    build_diag(DG, l, "fcw", blk, 31)
                    for (t0, T) in tiles_out:
                        pcv, pcvk = psum()
                        conv_mm(pcv[:, :T], DG, RAWc[blk], ('RAWc', blk), t0, T, 31, pcvk)
                        S.op('act', lambda e: e.activation(out=CV[:, blk, t0:t0 + T], in_=pcv[:, :T], func=AF.Identity,
                                                           bias=ppc("fcb", l, blk), scale=1.0), r=[pcvk, 'PP'], w=[('CV', blk, t0 // 512)])
                MEANs = [MEAN, sb("MEAN1", [128, 512], F32, pd_)]
                VARs = [VAR, sb("VAR1", [128, 512], F32, pd_)]
                CVBs = [CVB, [sb(f"CVBx{i}", [128, 512], BF16, pd_) for i in range(2)]]
                SQBs = [SQB, [sb(f"SQBx{i}", [128, 512], BF16, pd_) for i in range(2)]]
                TFs = [TF, [sb(f"TFx{i}", [128, 512], F32, pd_) for i in range(2)]]

                def cf_ln(t0, T, q):
                    MEAN_, VAR_, CVB_, SQB_, TF_ = MEANs[q], VARs[q], CVBs[q], SQBs[q], TFs[q]
                    tt = t0 // 512
                    p1, p1k = psum()
                    p2, p2k = psum()
                    for blk in range(2):
                        S.op('dve', lambda e: e.tensor_copy(out=CVB_[blk][:, :T], in_=CV[:, blk, t0:t0 + T]),
                             r=[('CV', blk, tt)], w=[('CVB', q, blk)])
                        S.op('act', lambda e: e.activation(out=SQB_[blk][:, :T], in_=CV[:, blk, t0:t0 + T], func=AF.Square),
                             r=[('CV', blk, tt)], w=[('SQB', q, blk)])
                    yield
                    for blk in range(2):
                        S.op('pe', lambda e: e.matmul(p1[:, :T], ONESB[:], CVB_[blk][:, :T], start=(blk == 0), stop=(blk == 1)),
                             r=[('CVB', q, blk), 'ONESB'], w=[p1k])
                    for blk in range(2):
                        S.op('pe', lambda e: e.matmul(p2[:, :T], ONESB[:], SQB_[blk][:, :T], start=(blk == 0), stop=(blk == 1)),
                             r=[('SQB', q, blk), 'ONESB'], w=[p2k])
                    yield
                    S.op('act', lambda e: e.activation(out=MEAN_[:, :T], in_=p1[:, :T], func=AF.Copy, scale=1.0 / 256), r=[p1k], w=[('MEAN', q)])
                    yield
                    S.op('dve', lambda e: e.tensor_tensor(out=VAR_[:, :T], in0=MEAN_[:, :T], in1=MEAN_[:, :T], op=ALU.mult),
                         r=[('MEAN', q)], w=[('VAR', q)])
                    yield
                    S.op('dve', lambda e: e.scalar_tensor_tensor(out=VAR_[:, :T], in0=p2[:, :T], scalar=1.0 / 256, in1=VAR_[:, :T],
                                                                 op0=ALU.mult, op1=ALU.subtract), r=[p2k, ('VAR', q)], w=[('VAR', q)])
                    yield
                    S.op('act', lambda e: e.activation(out=VAR_[:, :T], in_=VAR_[:, :T], func=AF.Sqrt, bias=EPS5[:, 0:1], scale=1.0),
                         r=[('VAR', q), 'EPS6'], w=[('VAR', q)])
                    yield
                    S.op('dve', lambda e: e.reciprocal(out=VAR_[:, :T], in_=VAR_[:, :T]), r=[('VAR', q)], w=[('VAR', q)])
                    yield
                    for blk in range(2):
                        S.op('dve', lambda e: e.tensor_tensor(out=TF_[blk][:, :T], in0=CV[:, blk, t0:t0 + T], in1=MEAN_[:, :T],
                                                              op=ALU.subtract), r=[('CV', blk, tt), ('MEAN', q)], w=[('TFc', q, blk)])
                    yield
                    for blk in range(2):
                        S.op('dve', lambda e: e.tensor_tensor(out=TF_[blk][:, :T], in0=TF_[blk][:, :T], in1=VAR_[:, :T], op=ALU.mult),
                             r=[('TFc', q, blk), ('VAR', q)], w=[('TFc', q, blk)])
                    yield
                    for blk in range(2):
                        S.op('act', lambda e: e.activation(out=BR[:, blk, t0:t0 + T], in_=TF_[blk][:, :T], func=AF.Silu,
                                                           bias=ppc("fnb", l, blk), scale=ppc("fng", l, blk)),
                             r=[('TFc', q, blk), 'PP'], w=brkeys(t0, T))

                for i0 in range(0, len(tiles_out), 2):
                    active = [cf_ln(t0, T, q) for q, (t0, T) in enumerate(tiles_out[i0:i0 + 2])]
                    while active:
                        for g_ in list(active):
                            try:
                                next(g_)
                            except StopIteration:
                                active.remove(g_)
                S.barrier()
            if debug == 'cf' and l == 0:
                dump([(BR[:, g, :], LT) for g in range(2)])
                return 'stop'
            out_proj(l, 3, BR, WO, sl_wo, tiles_out, j)
            S.barrier()
        return None

    moe_ctx = {}

    def ffn_phase(l, j, tiles, moe):
        SEL8B = moe_ctx.get('SEL8B')
        with ExitStack() as ph:
            NST = 2
            W1 = [sb(f"W1_{i}", [128, 8, 512], BF16, ph) for i in range(NST)]
            W3 = [sb(f"W3_{i}", [128, 8, 512], BF16, ph) for i in range(NST)]
            W2 = [sb(f"W2_{i}", [128, 4, 1024], BF16, ph) for i in range(NST)]
            sl_w = [S.slot(f"ffw{i}") for i in range(NST)]
            G = sb("G", [128, 4, L if moe else LT], BF16, ph)
            SA = [sb(f"SA{i}", [128, 512], F32, ph) for i in range(2)]
            CMB = sb("CMB", [128, L], BF16, ph) if moe else None
            nst = [0]
            nq = [0]

            def run(w1ap, w3ap, w2ap, F):
                f0 = 0
                while f0 < F:
                    fw = min(512, F - f0)
                    nb = fw // 128
                    s_ = nst[0] % NST
                    nst[0] += 1
                    S.dma('pool', sl_w[s_],
                          [(W1[s_][:, :, 0:fw], w1ap[:, f0:f0 + fw].rearrange("(k p) n -> p k n", p=128)),
                           (W3[s_][:, :, 0:fw], w3ap[:, f0:f0 + fw].rearrange("(k p) n -> p k n", p=128)),
                           (W2[s_][:, 0:nb, :], w2ap[f0:f0 + fw, :].rearrange("(k p) n -> p k n", p=128))],
                          w=[('FW', s_)])
                    for (t0, T) in tiles:
                        tt = t0 // 512
                        for jb in range(nb):
                            q = nq[0] % 2
                            nq[0] += 1
                            pa_, pak = psum()
                            for k in range(8):
                                S.op('pe', lambda e: e.matmul(pa_[:, :T], W1[s_][:, k, jb * 128:(jb + 1) * 128], H[:, k, t0:t0 + T],
                                                              start=(k == 0), stop=(k == 7)), r=[('FW', s_), ('H', k, tt)], w=[pak], inc=(k == 7))
                            pb2, pbk = psum()
                            for k in range(8):
                                S.op('pe', lambda e: e.matmul(pb2[:, :T], W3[s_][:, k, jb * 128:(jb + 1) * 128], H[:, k, t0:t0 + T],
                                                              start=(k == 0), stop=(k == 7)), r=[('FW', s_), ('H', k, tt)], w=[pbk], inc=(k == 7))
                            S.op('act', lambda e: e.activation(out=SA[q][:, :T], in_=pa_[:, :T], func=AF.Silu), r=[pak], w=[('SA', q)])
                            S.op('dve', lambda e: e.tensor_tensor(out=G[:, jb, t0:t0 + T], in0=SA[q][:, :T], in1=pb2[:, :T],
                                                                  op=ALU.mult), r=[('SA', q), pbk], w=[('G', jb, tt)])
                            if moe:
                                S.op('dve', lambda e: e.tensor_tensor(out=G[:, jb, t0:t0 + T], in0=G[:, jb, t0:t0 + T],
                                                                      in1=CMB[:, t0:t0 + T], op=ALU.mult),
                                     r=[('G', jb, tt), 'CMB'], w=[('G', jb, tt)])
                    for (t0, T) in tiles:
                        tt = t0 // 512
                        jj = j if t0 < L else 2
                        for kb in range(8):
                            po, pok = psum()
                            for jb in range(nb):
                                S.op('pe', lambda e: e.matmul(po[:, :T], W2[s_][:, jb, kb * 128:(kb + 1) * 128], G[:, jb, t0:t0 + T],
                                                              start=(jb == 0), stop=(jb == nb - 1)), r=[('FW', s_), ('G', jb, tt)], w=[pok], inc=(jb == nb - 1))
                            g_ap = MOD[:, l, 40 + kb, jj:jj + 1]
                            S.op('dve', lambda e: e.scalar_tensor_tensor(out=X[:, kb, t0:t0 + T], in0=po[:, :T], scalar=g_ap,
                                                                         in1=X[:, kb, t0:t0 + T], op0=ALU.mult, op1=ALU.add),
                                 r=[pok, ('X', kb, tt)], w=[('X', kb, tt)])
                    f0 += fw

            if not moe:
                run(dd("ffn_w1")[0], dd("ffn_w3")[0], dd("ffn_w2")[0], DFF)
            else:
                CMBT = moe
                for ex in range(NE):
                    for (t0, T) in tiles:
                        pc, pck = psum()
                        S.op('pe', lambda e: e.matmul(pc[:, :T], SEL8B[0:8, ex * 128:(ex + 1) * 128], CMBT[:, t0:t0 + T],
                                                      start=True, stop=True), r=['CMBT', 'SEL8B'], w=[pck])
                        S.op('act', lambda e: e.activation(out=CMB[:, t0:t0 + T], in_=pc[:, :T], func=AF.Copy), r=[pck], w=['CMB'])
                    run(dd("moe_w1")[0, ex], dd("moe_w3")[0, ex], dd("moe_w2")[0, ex], EFF)
            S.barrier()

    def route_phase(pl, plk, CMBT):
        with ExitStack() as ph:
            LG = sb("LG", [128, 16, 8], F32, ph)
            LG2 = sb("LG2", [128, 16, 8], F32, ph)
            EQ1 = sb("EQ1", [128, 16, 8], F32, ph)
            EQ2 = sb("EQ2", [128, 16, 8], F32, ph)
            M1 = sb("M1", [128, 16], F32, ph)
            M2 = sb("M2", [128, 16], F32, ph)
            P1 = sb("P1", [128, 16], F32, ph)
            P2 = sb("P2", [128, 16], F32, ph)
            CM = sb("CM", [128, 16, 8], F32, ph)
            bc = lambda t: t[:].unsqueeze(2).to_broadcast([128, 16, 8])
            S.op('dve', lambda e: e.tensor_copy(out=LG[:], in_=pl[:, 0:128].rearrange("p (s x) -> p s x", x=8)), r=[plk], w=['LG'])
            S.op('dve', lambda e: e.reduce_max(out=M1[:], in_=LG[:], axis=AX.X), r=['LG'], w=['M1'])
            S.op('dve', lambda e: e.tensor_tensor(out=EQ1[:], in0=LG[:], in1=bc(M1), op=ALU.is_equal), r=['LG', 'M1'], w=['EQ1'])
            S.op('dve', lambda e: e.scalar_tensor_tensor(out=LG2[:], in0=EQ1[:], scalar=-1e30, in1=LG[:], op0=ALU.mult, op1=ALU.add),
                 r=['EQ1', 'LG'], w=['LG2'])
            S.op('dve', lambda e: e.reduce_max(out=M2[:], in_=LG2[:], axis=AX.X), r=['LG2'], w=['M2'])
            S.op('dve', lambda e: e.tensor_tensor(out=EQ2[:], in0=LG2[:], in1=bc(M2), op=ALU.is_equal), r=['LG2', 'M2'], w=['EQ2'])
            S.op('dve', lambda e: e.tensor_tensor(out=M2[:], in0=M2[:], in1=M1[:], op=ALU.subtract), r=['M2', 'M1'], w=['M2'])
            S.op('act', lambda e: e.activation(out=P2[:], in_=M2[:], func=AF.Sigmoid), r=['M2'], w=['P2'])
            S.op('act', lambda e: e.activation(out=P1[:], in_=M2[:], func=AF.Sigmoid, scale=-1.0), r=['M2'], w=['P1'])
            S.op('dve', lambda e: e.tensor_tensor(out=EQ1[:], in0=EQ1[:], in1=bc(P1), op=ALU.mult), r=['EQ1', 'P1'], w=['EQ1'])
            S.op('dve', lambda e: e.tensor_tensor(out=EQ2[:], in0=EQ2[:], in1=bc(P2), op=ALU.mult), r=['EQ2', 'P2'], w=['EQ2'])
            S.op('dve', lambda e: e.tensor_tensor(out=CM[:], in0=EQ1[:], in1=EQ2[:], op=ALU.add), r=['EQ1', 'EQ2'], w=['CM'])
            for grp in range(4):
                pt, ptk = psum()
                for s4 in range(4):
                    sub = grp * 4 + s4
                    S.op('pe', lambda e: e.transpose(pt[0:8, s4 * 128:(s4 + 1) * 128], CM[:, sub, :], CST[:, C_ID:C_ID + 128]),
                         r=['CM', 'CST'], w=[ptk])
                S.op('act', lambda e: e.activation(out=CMBT[:, grp * 512:(grp + 1) * 512], in_=pt[0:8, :], func=AF.Copy),
                     r=[ptk], w=['CMBT'])
            S.barrier()


    def final_phase(b):
        with ExitStack() as ph:
            SQ = [sb(f"fSQ{i}", [128, 512], BF16, ph) for i in range(2)]
            RS = [sb(f"fRS{i}", [128, 512], F32, ph) for i in range(2)]
            OT = [sb(f"fOT{i}", [128, 8, 512], F32, ph) for i in range(2)]
            nq = 0
            for ti, (t0, T) in enumerate(TT_MAIN):
                tt = t0 // 512
                pss, pk = psum()
                for k in range(8):
                    q = nq % 2
                    nq += 1
                    S.op('act', lambda e: e.activation(out=SQ[q][:, :T], in_=X[:, k, t0:t0 + T], func=AF.Square),
                         r=[('X', k, tt)], w=[('fSQ', q)])
                    S.op('pe', lambda e: e.matmul(pss[:, :T], ONESB[:], SQ[q][:, :T], start=(k == 0), stop=(k == 7)),
                         r=[('fSQ', q), 'ONESB'], w=[pk])
                rq = ti % 2
                S.op('act', lambda e: e.activation(out=RS[rq][:, :T], in_=pss[:, :T], func=AF.Sqrt, bias=EPS6[:, 0:1],
                                                   scale=1.0 / D), r=[pk, 'EPS6'], w=[('fRS', rq)])
                S.op('dve', lambda e: e.reciprocal(out=RS[rq][:, :T], in_=RS[rq][:, :T]), r=[('fRS', rq)], w=[('fRS', rq)])
                for k in range(8):
                    g_ap = ppc("fin", 0, k)
                    S.op('dve', lambda e: e.scalar_tensor_tensor(out=OT[rq][:, k, :T], in0=X[:, k, t0:t0 + T], scalar=g_ap,
                                                                 in1=RS[rq][:, :T], op0=ALU.mult, op1=ALU.mult),
                         r=[('X', k, tt), ('fRS', rq), 'PP'], w=[('fOT', rq)])
                S.dma('sp', sl_out[rq], [(outT[b, :, t0:t0 + T].rearrange("(k p) t -> p k t", p=128), OT[rq][:, :, :T])],
                      r=[('fOT', rq)])
            if b + 1 < nseq:
                load_x(b + 1)
            S.barrier()

    import os
    if os.environ.get("OPLIMIT"):
        S.limit = int(os.environ["OPLIMIT"])
    for b in range(nseq):
      try:
          norm_phase(0, 0, b, TT_ALL)
          if debug == 'norm1':
              dump([(H[:, k, :], LT) for k in range(8)])
              break
          r_ = mixer_phase(0, b, False)
          if r_ == 'stop':
              break
          if debug == 'mix0':
              dump([(X[:, k, :], LT) for k in range(8)])
              break
          norm_phase(0, 1, b, TT_ALL)
          ffn_phase(0, b, TT_ALL, None)
          if debug == 'l0':
              dump([(X[:, k, :], LT) for k in range(8)])
              break
          norm_phase(1, 0, b, TT_ALL)
          r_ = mixer_phase(1, b, True)
          if debug == 'mix1':
              dump([(X[:, k, :], LT) for k in range(8)])
              break
          pl, plk = PS[5], ('ps', 5)
          st['ps'] = 0
          with ExitStack() as mo:
              CMBT = sb("CMBT", [8, L], BF16, mo)
              SEL8B = sb("SEL8B", [8, 1024], BF16, mo)
              S.dma('pool', sl_sel, [(SEL8B[:], cstd[0:8, C_S8:C_S8 + 1024])], w=['SEL8B'])
              norm_phase(1, 1, b, TT_MAIN, moe=(pl, plk))
              route_phase(pl, plk, CMBT)
              moe_ctx['SEL8B'] = SEL8B
              ffn_phase(1, b, TT_MAIN, CMBT)
          final_phase(b)
      except StopIteration:
        S.limit = None
        print('STOPPED at nops', S.nops)
        dump([(X[:, 0, :], LT)])
        break

    S.barrier()
    global USED
    USED = set(DECL.keys())
    return nc


def _host_layout(inp):
    f = lambda a: np.ascontiguousarray(np.asarray(a, dtype=np.float32))
    pp = np.zeros((128, NPP), np.float32)

    def blk(v):
        v = np.asarray(v, np.float32)
        return v.reshape(-1, 128).T

    for l in range(2):
        def put(name, arr):
            o = PPO[(name, l)]
            pp[:, o:o + arr.shape[1]] = arr
        put("n1g", blk(inp['norm1_g'][l]))
        put("n2g", blk(inp['norm2_g'][l]))
        cw = np.asarray(inp['ssd_conv_w'][l], np.float32)
        put("scw", np.stack([blk(cw[k]) for k in range(5)], axis=2).reshape(128, 30))
        put("scb", blk(inp['ssd_conv_b'][l]))
        put("sD", blk(np.repeat(np.asarray(inp['ssd_d'][l], np.float32), 64)))
        put("sng", blk(inp['ssd_norm_g'][l]))
        c3 = np.asarray(inp['sc_conv_w'][l], np.float32)
        put("ccw", np.stack([blk(c3[k]) for k in range(3)], axis=2).reshape(128, 6))
        c31 = np.asarray(inp['cf_conv_w'][l], np.float32)
        put("fcw", np.stack([blk(c31[k]) for k in range(31)], axis=2).reshape(128, 62))
        put("fcb", blk(inp['cf_conv_b'][l]))
        put("fng", blk(inp['cf_norm_g'][l]))
        put("fnb", blk(inp['cf_norm_b'][l]))
        put("gbs", np.asarray(inp['gm_bs'][l], np.float32).T)
        put("modb", blk(inp['mod_b'][l]))
    o = PPO[("fin", 0)]
    pp[:, o:o + 8] = blk(inp['final_norm_g'])
    rb = np.zeros((128, 2 * RBW), np.float32)
    for l in range(2):
        row = np.concatenate([np.asarray(inp['ssd_dt_bias'][l], np.float32).reshape(8),
                              np.asarray(inp['ssd_a_log'][l], np.float32).reshape(8),
                              np.asarray(inp['gm_norm_g'][l], np.float32), np.asarray(inp['gm_norm_b'][l], np.float32)])
        rb[:, l * RBW:(l + 1) * RBW] = row[None, :]
    cst = np.zeros((128, NCST), np.float32)
    i = np.arange(128)
    cst[:, C_ID:C_ID + 128] = np.eye(128)
    cst[:, C_TU:C_TU + 128] = (i[:, None] <= i[None, :])
    cst[:, C_TL:C_TL + 128] = (i[:, None] >= i[None, :])
    cst[:, C_MF:C_MF + 128] = np.where(i[None, :] >= i[:, None], 0.0, -1e30)
    cst[:, C_MB:C_MB + 128] = np.where(i[None, :] <= i[:, None], 0.0, -1e30)
    cst[:, C_ON:C_ON + 128] = 1.0
    for h in range(4):
        cst[h, C_S4 + h * 128:C_S4 + (h + 1) * 128] = 1.0
    for e in range(8):
        cst[e, C_S8 + e * 128:C_S8 + (e + 1) * 128] = 1.0
    rt = np.ascontiguousarray(np.asarray(inp['moe_router'][0], np.float32).reshape(8, 128, 8).transpose(1, 0, 2))
    ws = np.asarray(inp['gm_ws'], np.float32)
    wsT = np.ascontiguousarray(ws.transpose(3, 0, 1, 2)).reshape(128, 2 * 4 * 128)
    shared = {"pp": pp, "rb": rb, "cst": cst, "rt": rt, "wsT": wsT}
    for kname in ("mod_w", "w_in", "w_out", "ffn_w1", "ffn_w3", "ffn_w2", "moe_w1", "moe_w3", "moe_w2"):
        shared[kname] = f(inp[kname])
    return shared


def _core_inputs(inp, shared, core, nseq=2):
    x = np.asarray(inp['x'], np.float32)
    ctx = np.asarray(inp['ctx'], np.float32)
    c = np.asarray(inp['c'], np.float32)
    cc = np.asarray(inp['c_ctx'], np.float32)
    bs = [core * 2 + i for i in range(nseq)]
    m = dict(shared)
    m["xT"] = np.ascontiguousarray(np.stack([x[b].T for b in bs]))
    m["cxT"] = np.ascontiguousarray(np.stack([ctx[b].T for b in bs]))
    cols = [c[core * 2], c[core * 2 + 1], cc]
    m["cT"] = np.ascontiguousarray(np.stack([v.reshape(8, 128).T for v in cols], axis=2))
    return m


def kernel(**inp):
    shared = _host_layout(inp)
    nc = build(2)
    in_maps = [_core_inputs(inp, shared, c) for c in range(8)]
    res = run_bass_kernel_spmd(nc, in_maps, core_ids=list(range(8)))
    out = np.empty((16, L, D), np.float32)
    for c in range(8):
        o = res.results[c]["outT"]
        for i in range(2):
            out[c * 2 + i] = o[i].T
    return out
```
